# Optimizing a Trainium2 kernel written in Bass

```python
import math
import jax, jax.numpy as jnp
from jax import lax
import numpy as np

D_MODEL = 1024
BATCH = 1
SEQ = 16384
DEPTH = 2

HEAD_DIM = 64
MOBA_HEADS = 8
DSA_HEADS = 8
N_HEADS = MOBA_HEADS + DSA_HEADS
MIX_WIDTH = N_HEADS * HEAD_DIM
MOBA_WIDTH = MOBA_HEADS * HEAD_DIM
DSA_WIDTH = DSA_HEADS * HEAD_DIM
MOBA_BLOCK = 256
MOBA_TOPK = 3
IDX_HEADS = 4
IDX_DIM = 64
DSA_TOPK_MAX = 256
N_BUCKETS = 32
MAX_DISTANCE = 4096
PEER_HEADS = 8
PEER_NKEYS = 128
PEER_EXPERTS = PEER_NKEYS * PEER_NKEYS
PEER_DKEY = 256
PEER_TOPK = 16
Q_CHUNK = 128
T_CHUNK = 128
EPS = 1e-6
PROJ_SIZES = (MOBA_WIDTH, MOBA_WIDTH, MOBA_WIDTH, DSA_WIDTH, DSA_WIDTH, DSA_WIDTH,
              IDX_HEADS * IDX_DIM, IDX_DIM, IDX_HEADS)
PROJ_WIDTH = 3 * MOBA_WIDTH + 3 * DSA_WIDTH + IDX_HEADS * IDX_DIM + IDX_DIM + IDX_HEADS

kernel_name = 'hybrid_moba_dsa_peer_adaln'


def rms_norm(x, g):
    xf = x.astype(jnp.float32)
    y = xf * lax.rsqrt(jnp.mean(xf * xf, axis=-1, keepdims=True) + EPS)
    return (y * g).astype(x.dtype)


def modulate(h, shift, scale):
    return h * (1 + scale[:, None, :]) + shift[:, None, :]


def t5_bucket(dist):
    max_exact = N_BUCKETS // 2
    d = jnp.maximum(dist, 0)
    df = jnp.maximum(d, 1).astype(jnp.float32)
    large = max_exact + (jnp.log(df / max_exact) / math.log(MAX_DISTANCE / max_exact)
                         * (N_BUCKETS - max_exact)).astype(jnp.int32)
    return jnp.where(d < max_exact, d, jnp.minimum(large, N_BUCKETS - 1))


def moba_attention(q, k, v, tab):
    B, H, S, Dh = q.shape
    nb = -(-S // MOBA_BLOCK)
    pad = nb * MOBA_BLOCK - S
    kp = jnp.pad(k, ((0, 0), (0, 0), (0, pad), (0, 0))).reshape(B, H, nb, MOBA_BLOCK, Dh)
    vp = jnp.pad(v, ((0, 0), (0, 0), (0, pad), (0, 0))).reshape(B, H, nb, MOBA_BLOCK, Dh)
    k_mean = jnp.mean(kp.astype(jnp.float32), axis=3)
    n_sel = max(min(MOBA_TOPK, nb - 1), 1)
    scale = HEAD_DIM ** -0.5
    bi = jnp.arange(B)[:, None, None, None]
    hi = jnp.arange(H)[None, :, None, None]
    blk = jnp.arange(MOBA_BLOCK)

    def one_chunk(ci):
        t0 = ci * Q_CHUNK
        t = t0 + jnp.arange(Q_CHUNK)
        b0 = t0 // MOBA_BLOCK
        qc = lax.dynamic_slice_in_dim(q, t0, Q_CHUNK, axis=2)
        gate = jnp.einsum('bhqd,bhnd->bhqn', qc.astype(jnp.float32), k_mean)
        gate = jnp.where(jnp.arange(nb) < b0, gate, -jnp.inf)
        _, sel = lax.top_k(gate, n_sel)
        sel_ok = sel < b0
        kg = kp[bi, hi, sel]
        vg = vp[bi, hi, sel]
        s_sel = jnp.einsum('bhqd,bhqjkd->bhqjk', qc, kg).astype(jnp.float32) * scale
        pos_sel = sel[..., None] * MOBA_BLOCK + blk
        s_sel = s_sel + tab[hi[..., None], t5_bucket(t[:, None, None] - pos_sel)]
        s_sel = jnp.where(sel_ok[..., None], s_sel, -jnp.inf).reshape(B, H, Q_CHUNK, n_sel * MOBA_BLOCK)
        ko = lax.dynamic_index_in_dim(kp, b0, axis=2, keepdims=False)
        vo = lax.dynamic_index_in_dim(vp, b0, axis=2, keepdims=False)
        pos_own = b0 * MOBA_BLOCK + blk
        s_own = jnp.einsum('bhqd,bhkd->bhqk', qc, ko).astype(jnp.float32) * scale
        s_own = s_own + jnp.take(tab, t5_bucket(t[:, None] - pos_own[None, :]), axis=1)[None]
        s_own = jnp.where(pos_own[None, :] <= t[:, None], s_own, -jnp.inf)
        p = jax.nn.softmax(jnp.concatenate([s_sel, s_own], axis=-1), axis=-1).astype(v.dtype)
        p_sel = p[..., :n_sel * MOBA_BLOCK].reshape(B, H, Q_CHUNK, n_sel, MOBA_BLOCK)
        p_own = p[..., n_sel * MOBA_BLOCK:]
        return (jnp.einsum('bhqjk,bhqjkd->bhqd', p_sel, vg)
                + jnp.einsum('bhqk,bhkd->bhqd', p_own, vo))

    out = lax.map(one_chunk, jnp.arange(S // Q_CHUNK))
    return jnp.transpose(out, (1, 2, 0, 3, 4)).reshape(B, H, S, Dh)


def dsa_attention(q, k, v, q_idx, k_idx, w_idx, tab):
    B, S, H, Dh = q.shape
    topk = min(DSA_TOPK_MAX, S // 4)
    scale = HEAD_DIM ** -0.5
    keys = jnp.arange(S)
    bi = jnp.arange(B)[:, None, None]

    def one_chunk(ci):
        t0 = ci * Q_CHUNK
        t = t0 + jnp.arange(Q_CHUNK)
        qi = lax.dynamic_slice_in_dim(q_idx, t0, Q_CHUNK, axis=1)
        wi = lax.dynamic_slice_in_dim(w_idx, t0, Q_CHUNK, axis=1)
        qc = lax.dynamic_slice_in_dim(q, t0, Q_CHUNK, axis=1)
        rel = jax.nn.relu(jnp.einsum('bqhd,bsd->bqhs', qi, k_idx).astype(jnp.float32))
        score = jnp.einsum('bqh,bqhs->bqs', wi.astype(jnp.float32), rel)
        score = jnp.where(keys[None, None, :] <= t[None, :, None], score, -jnp.inf)
        _, idx = lax.top_k(score, topk)
        kg = k[bi, idx]
        vg = v[bi, idx]
        s = jnp.einsum('bqhd,bqkhd->bhqk', qc, kg).astype(jnp.float32) * scale
        bias = jnp.moveaxis(jnp.take(tab, t5_bucket(t[None, :, None] - idx), axis=1), 0, 1)
        s = jnp.where((idx <= t[None, :, None])[:, None], s + bias, -jnp.inf)
        p = jax.nn.softmax(s, axis=-1).astype(v.dtype)
        return jnp.einsum('bhqk,bqkhd->bqhd', p, vg)

    out = lax.map(one_chunk, jnp.arange(S // Q_CHUNK))
    return jnp.transpose(out, (1, 0, 2, 3, 4)).reshape(B, S, H * Dh)


def hybrid_mixer(h, w_in, w_out, rel_bias):
    B, S, _ = h.shape
    proj = h @ w_in
    parts = []
    off = 0
    for size in PROJ_SIZES:
        parts.append(proj[..., off:off + size])
        off += size
    mq, mk, mv, dq, dk, dv, iq, ik, iw = parts
    to_bhsd = lambda t, nh: jnp.transpose(t.reshape(B, S, nh, HEAD_DIM), (0, 2, 1, 3))
    to_bshd = lambda t, nh: t.reshape(B, S, nh, HEAD_DIM)
    y_moba = moba_attention(to_bhsd(mq, MOBA_HEADS), to_bhsd(mk, MOBA_HEADS),
                            to_bhsd(mv, MOBA_HEADS), rel_bias[:MOBA_HEADS])
    y_moba = jnp.transpose(y_moba, (0, 2, 1, 3)).reshape(B, S, MOBA_WIDTH)
    ikf = ik.astype(jnp.float32)
    ik_n = (ikf * lax.rsqrt(jnp.mean(ikf * ikf, axis=-1, keepdims=True) + EPS)).astype(ik.dtype)
    y_dsa = dsa_attention(to_bshd(dq, DSA_HEADS), to_bshd(dk, DSA_HEADS), to_bshd(dv, DSA_HEADS),
                          iq.reshape(B, S, IDX_HEADS, IDX_DIM), ik_n, iw * IDX_HEADS ** -0.5,
                          rel_bias[MOBA_HEADS:])
    y = jnp.concatenate([y_moba, y_dsa], axis=-1)
    return y @ w_out


def peer_ffn(h, w_query, sub_keys, u_emb, v_emb):
    B, S, D = h.shape

    def one_chunk(ci):
        hc = lax.dynamic_slice_in_dim(h, ci * T_CHUNK, T_CHUNK, axis=1)
        q = (hc @ w_query).reshape(B, T_CHUNK, PEER_HEADS, 2, PEER_DKEY // 2)
        s = jnp.einsum('bthpd,hpnd->bthpn', q, sub_keys).astype(jnp.float32)
        s_top, i_top = lax.top_k(s, PEER_TOPK)
        cand = (s_top[..., 0, :, None] + s_top[..., 1, None, :]).reshape(B, T_CHUNK, PEER_HEADS, PEER_TOPK * PEER_TOPK)
        cand_idx = (i_top[..., 0, :, None] * PEER_NKEYS + i_top[..., 1, None, :]).reshape(cand.shape)
        g_s, pos = lax.top_k(cand, PEER_TOPK)
        e_idx = jnp.take_along_axis(cand_idx, pos, axis=-1)
        g = jax.nn.softmax(g_s, axis=-1)
        u = u_emb[e_idx]
        act = jax.nn.gelu(jnp.einsum('btd,bthkd->bthk', hc, u).astype(jnp.float32), approximate=False)
        return jnp.einsum('bthk,bthkd->btd', (g * act).astype(h.dtype), v_emb[e_idx])

    out = lax.map(one_chunk, jnp.arange(S // T_CHUNK))
    return jnp.transpose(out, (1, 0, 2, 3)).reshape(B, S, D)


def setup_inputs(seed: int = 0) -> dict:
    key = jax.random.key(seed)
    ks = jax.random.split(key, 14)
    f32 = jnp.float32
    nrm = lambda k, shape, s: jax.random.normal(k, shape, f32) * s
    return {
        'x': nrm(ks[0], (BATCH, SEQ, D_MODEL), 1.0),
        'c': nrm(ks[1], (BATCH, D_MODEL), 1.0),
        'w_ada': nrm(ks[2], (DEPTH, D_MODEL, 6 * D_MODEL), 0.5 * D_MODEL ** -0.5),
        'b_ada': nrm(ks[3], (DEPTH, 6 * D_MODEL), 0.02),
        'norm_attn': 1.0 + nrm(ks[4], (DEPTH, D_MODEL), 0.02),
        'norm_ffn': 1.0 + nrm(ks[5], (DEPTH, D_MODEL), 0.02),
        'w_in': nrm(ks[6], (DEPTH, D_MODEL, PROJ_WIDTH), D_MODEL ** -0.5),
        'w_out': nrm(ks[7], (DEPTH, MIX_WIDTH, D_MODEL), MIX_WIDTH ** -0.5),
        'rel_bias': nrm(ks[8], (N_HEADS, N_BUCKETS), 0.5),
        'peer_wq': nrm(ks[9], (DEPTH, D_MODEL, PEER_HEADS * PEER_DKEY), D_MODEL ** -0.5),
        'peer_subkeys': nrm(ks[10], (DEPTH, PEER_HEADS, 2, PEER_NKEYS, PEER_DKEY // 2), (PEER_DKEY // 2) ** -0.5),
        'peer_u': nrm(ks[11], (DEPTH, PEER_EXPERTS, D_MODEL), D_MODEL ** -0.5),
        'peer_v': nrm(ks[12], (DEPTH, PEER_EXPERTS, D_MODEL), PEER_HEADS ** -0.5),
        'norm_final': 1.0 + nrm(ks[13], (D_MODEL,), 0.02),
    }


def reference(x, c, w_ada, b_ada, norm_attn, norm_ffn, w_in, w_out, rel_bias,
              peer_wq, peer_subkeys, peer_u, peer_v, norm_final):
    mod = jnp.einsum('bd,lde->lbe', jax.nn.silu(c), w_ada) + b_ada[:, None, :]
    for l in range(DEPTH):
        sh1, sc1, g1, sh2, sc2, g2 = jnp.split(mod[l], 6, axis=-1)
        h = modulate(rms_norm(x, norm_attn[l]), sh1, sc1)
        x = x + g1[:, None, :] * hybrid_mixer(h, w_in[l], w_out[l], rel_bias)
        h = modulate(rms_norm(x, norm_ffn[l]), sh2, sc2)
        x = x + g2[:, None, :] * peer_ffn(h, peer_wq[l], peer_subkeys[l], peer_u[l], peer_v[l])
    return rms_norm(x, norm_final)
```

```python
import numpy as np
import ml_dtypes
import concourse.bass as bass
import concourse.mybir as mybir
from concourse.bass_utils import run_bass_kernel_spmd

F32 = mybir.dt.float32
BF16 = mybir.dt.bfloat16
U32 = mybir.dt.uint32
AF = mybir.ActivationFunctionType
ALU = mybir.AluOpType
AX = mybir.AxisListType
NPBF = ml_dtypes.bfloat16

NCORES = 8
D = 1024
S = 16384
NCH = S // 128
CPC = NCH // NCORES
TPC = S // NCORES
PW = 3396
EPS = 1e-6
NEG = -30000.0


class T:
    def __init__(self, h, name="", track=True):
        self.h = h
        self.name = name
        self.track = track
        self.w = None
        self.r = []

    def __getitem__(self, k):
        return self.h[k]


class P:
    def __init__(self, nc):
        self.nc = nc
        self.eng = {}
        self.stack = []
        for nm, h in (("pe", nc.tensor), ("act", nc.scalar), ("dve", nc.vector), ("pool", nc.gpsimd)):
            sem = self._sem("s_" + nm)
            self.eng[nm] = dict(h=h, sem=sem, n=0, waited={}, name=nm)
        self.q = {}
        for nm, h in (("sp", nc.sync), ("poolq", nc.gpsimd), ("actq", nc.scalar)):
            sems = [self._sem(f"d_{nm}{i}") for i in range(8)]
            self.q[nm] = dict(h=h, sems=sems, n=0, name=nm)
        self.issuer = {"sp": dict(h=nc.sync, waited={}, name="sp_i"),
                       "poolq": self.eng["pool"], "actq": self.eng["act"]}

    def _sem(self, name):
        cm = self.nc.semaphore(name)
        s = cm.__enter__()
        self.stack.append(cm)
        return s

    def sb(self, name, shape, dt):
        self._uid = getattr(self, "_uid", 0) + 1
        name = f"{name}_{self._uid}"
        cm = self.nc.sbuf_tensor(name, shape, dt)
        h = cm.__enter__()
        self.stack.append(cm)
        return T(h, name)

    def ps(self, name, shape, dt):
        self._uid = getattr(self, "_uid", 0) + 1
        name = f"{name}_{self._uid}"
        cm = self.nc.psum_tensor(name, shape, dt)
        h = cm.__enter__()
        self.stack.append(cm)
        return T(h, name)

    def dram(self, name, shape, dt, kind):
        bind = getattr(self, "bind", {})
        if name in bind:
            return bind[name]
        if kind == "Internal":
            self._uid = getattr(self, "_uid", 0) + 1
            name = f"{name}_{self._uid}"
        h = self.nc.dram_tensor(name, list(shape), dt, kind=kind).ap()
        return T(h, name, track=False)

    def _wait(self, e, dep):
        if dep is None:
            return
        if dep[0] == "e":
            _, en, n = dep
            if e.get("name") == en == "pe":
                return
            if e["waited"].get(en, 0) >= n:
                return
            e["h"].wait_ge(self.eng[en]["sem"], n)
            e["waited"][en] = n
        else:
            _, qn, si, val = dep
            key = (qn, si)
            if e["waited"].get(key, 0) >= val:
                return
            e["h"].wait_ge(self.q[qn]["sems"][si], val)
            e["waited"][key] = val

    def _deps(self, e, reads, writes):
        for t in reads:
            if t.track:
                self._wait(e, t.w)
        for t in writes:
            if not t.track:
                continue
            self._wait(e, t.w)
            for d in t.r:
                self._wait(e, d)

    def _mark(self, me, reads, writes):
        for t in reads:
            if not t.track:
                continue
            t.r.append(me)
            if len(t.r) > 64:
                t.r = t.r[-48:]
        for t in writes:
            if not t.track:
                continue
            t.w = me
            t.r = []

    def op(self, en, fn, reads=(), writes=()):
        e = self.eng[en]
        self._deps(e, reads, writes)
        ins = fn(e["h"])
        e["n"] += 1
        ins.then_inc(e["sem"], 1)
        self._mark(("e", en, e["n"]), reads, writes)
        return ins

    def dma(self, qn, out, in_, reads=(), writes=(), **kw):
        q = self.q[qn]
        e = self.issuer[qn]
        self._deps(e, reads, writes)
        i = q["n"]
        ns = len(q["sems"])
        si = i % ns
        val = 16 * (i // ns + 1)
        if val > 16:
            self._wait(e, ("d", qn, si, val - 16))
        ins = q["h"].dma_start(out=out, in_=in_, **kw)
        ins.then_inc(q["sems"][si], 16)
        q["n"] += 1
        self._mark(("d", qn, si, val), reads, writes)

    def scope_begin(self):
        if not hasattr(self, "_marks"):
            self._marks = []
        self._marks.append(len(self.stack))

    def barrier(self):
        waiters = list(self.eng.values()) + [self.issuer["sp"]]
        snap = {en: e["n"] for en, e in self.eng.items()}
        for w in waiters:
            for en, n in snap.items():
                if n > 0:
                    if w["waited"].get(en, 0) < n:
                        w["h"].wait_ge(self.eng[en]["sem"], n)
                        w["waited"][en] = n
            for qn, q in self.q.items():
                ns = len(q["sems"])
                for si in range(ns):
                    cnt = (q["n"] - si + ns - 1) // ns if q["n"] > si else 0
                    if cnt > 0:
                        self._wait(w, ("d", qn, si, 16 * cnt))

    def scope_end(self):
        self.barrier()
        mark = self._marks.pop()
        while len(self.stack) > mark:
            cm = self.stack.pop()
            cm.__exit__(None, None, None)

    def finish(self, tiles=()):
        e = self.issuer["sp"]
        for qn, q in self.q.items():
            ns = len(q["sems"])
            for si in range(ns):
                cnt = (q["n"] - si + ns - 1) // ns if q["n"] > si else 0
                if cnt > 0:
                    self._wait(e, ("d", qn, si, 16 * cnt))

    def mm(self, out_t, out_ap, lhsT_t, lhsT_ap, rhs_t, rhs_ap, start, stop):
        return self.op("pe", lambda e: e.matmul(out_ap, lhsT=lhsT_ap, rhs=rhs_ap, start=start, stop=stop),
                       reads=[lhsT_t, rhs_t], writes=[out_t])


def _trim(reads):
    return reads


def build_M():
    nc = bass.Bass("TRN2", target_bir_lowering=False)
    p = P(nc)
    W = 1536
    cT = p.dram("cT", [128, 8], F32, "ExternalInput")
    w = p.dram("w", [1024, W], F32, "ExternalInput")
    b = p.dram("b", [1, W], F32, "ExternalInput")
    o = p.dram("o", [1, W], F32, "ExternalOutput")
    cs = p.sb("cs", [128, 8], F32)
    sc = p.sb("sc", [128, 8], F32)
    ws = p.sb("ws", [128, 8, W], F32)
    bs = p.sb("bs", [1, W], F32)
    os_ = p.sb("os", [1, W], F32)
    acc = p.ps("acc", [1, W], F32)
    p.dma("sp", cs[:], cT[:, :], reads=[cT], writes=[cs])
    p.dma("sp", bs[:], b[:, :], reads=[b], writes=[bs])
    wv = w.h.rearrange("(k p) n -> p k n", p=128)
    for k in range(8):
        p.dma("sp" if k % 2 == 0 else "poolq", ws[:, k, :], wv[:, k, :], reads=[w], writes=[ws])
    p.op("act", lambda e: e.activation(out=sc[:], in_=cs[:], func=AF.Silu), reads=[cs], writes=[sc])
    for g in range(W // 512):
        for k in range(8):
            p.mm(acc, acc[:, g * 512:(g + 1) * 512], sc, sc[:, k:k + 1], ws, ws[:, k, g * 512:(g + 1) * 512],
                 start=(k == 0), stop=(k == 7))
    p.op("dve", lambda e: e.tensor_tensor(out=os_[:], in0=acc[:], in1=bs[:], op=ALU.add), reads=[acc, bs], writes=[os_])
    p.dma("sp", o[:, :], os_[:], reads=[os_], writes=[o])
    p.finish([o])
    return nc


def run_M(c, w_ada, b_ada):
    cT = np.ascontiguousarray(c.reshape(8, 128).T)
    wf = np.concatenate([w_ada[0], w_ada[1]], axis=1)
    bf = np.concatenate([b_ada[0], b_ada[1]], axis=0)[None]
    nc = build_M()
    maps = [{"cT": cT, "w": np.ascontiguousarray(wf[:, i * 1536:(i + 1) * 1536]),
             "b": np.ascontiguousarray(bf[:, i * 1536:(i + 1) * 1536])} for i in range(NCORES)]
    res = run_bass_kernel_spmd(nc, maps, core_ids=list(range(NCORES)))
    mod = np.concatenate([r["o"] for r in res.results], axis=1)[0]
    return mod.reshape(2, 6, 1024)


def emit_norm_hT(p, xs, A_row, B_row, hT, scr, ident_b, tp_ps, tag, out_ap=None):
    sq, ss, rs, hf, hb = scr["sq"], scr["ss"], scr["rs"], scr["hf"], scr["hb"]
    p.op("act", lambda e: e.activation(out=sq[:], in_=xs[:], func=AF.Square, accum_out=ss[:]),
         reads=[xs], writes=[sq, ss])
    p.op("dve", lambda e: e.tensor_scalar(out=rs[:], in0=ss[:], scalar1=1.0 / D, scalar2=EPS, op0=ALU.mult, op1=ALU.add),
         reads=[ss], writes=[rs])
    p.op("act", lambda e: e.activation(out=rs[:], in_=rs[:], func=AF.Sqrt), reads=[rs], writes=[rs])
    p.op("dve", lambda e: e.reciprocal(out=rs[:], in_=rs[:]), reads=[rs], writes=[rs])
    p.op("dve", lambda e: e.scalar_tensor_tensor(out=hf[:], in0=xs[:], scalar=rs[:, 0:1], in1=A_row[:], op0=ALU.mult, op1=ALU.mult),
         reads=[xs, rs, A_row], writes=[hf])
    p.op("pool", lambda e: e.tensor_tensor(out=hb[:], in0=hf[:], in1=B_row[:], op=ALU.add),
         reads=[hf, B_row], writes=[hb])
    for k in range(8):
        p.op("pe", lambda e: e.transpose(out=tp_ps[:, k, :], in_=hb[:, k * 128:(k + 1) * 128], identity=ident_b[:]),
             reads=[hb, ident_b], writes=[tp_ps])
    o_ap = hT[:] if out_ap is None else out_ap
    p.op("act", lambda e: e.activation(out=o_ap, in_=tp_ps[:], func=AF.Copy), reads=[tp_ps], writes=[hT])


def load_w_bf16(p, w_dram, wb, ncols, stage, qn="sp"):
    wv = w_dram.h.rearrange("(k p) n -> p k n", p=128)
    for k in range(8):
        st = stage[k % len(stage)]
        p.dma(qn, st[:, 0:ncols], wv[:, k, :], reads=[w_dram], writes=[st])
        eng = "pool" if k % 2 == 0 else "dve"
        p.op(eng, lambda e: e.tensor_copy(out=wb[:, k, 0:ncols], in_=st[:, 0:ncols]), reads=[st], writes=[wb])


def build_A(p=None, fused=None):
    standalone = p is None
    if standalone:
        nc = bass.Bass("TRN2", target_bir_lowering=False)
        p = P(nc)
    x = p.dram("x", [TPC, D], F32, "ExternalInput")
    nrm = p.dram("nrm", [128, D], F32, "ExternalInput")
    scr_ = p.dram("sc", [128, D], F32, "ExternalInput")
    shr_ = p.dram("sh", [128, D], F32, "ExternalInput")
    w = p.dram("w", [D, PW], F32, "ExternalInput")
    idb = p.dram("idb", [128, 128], BF16, "ExternalInput")
    if fused is None:
        of = p.dram("of", [TPC, PW], F32, "ExternalOutput")
        ob = p.dram("ob", [TPC, PW], BF16, "ExternalOutput")
    else:
        identf = p.sb("identf", [128, 128], F32)
        p.dma("sp", identf[:], fused["idf"][:, :], reads=[], writes=[identf])
        tpf_ps = p.ps("tpf_ps", [128, 512], F32)
        tpk_ps = p.ps("tpk_ps", [128, 1024], BF16)
        tsb = [p.sb(f"tsb{i}", [128, 512], F32) for i in range(2)]
        ksb = [p.sb(f"ksb{i}", [128, 512], BF16) for i in range(2)]
        vaug = [p.sb(f"vaug{i}", [128, 8, 65], BF16) for i in range(2)]
        for i in range(2):
            p.op("pool", lambda e: e.memset(vaug[i][:], 1.0), writes=[vaug[i]])
        tsi = 0
        ksi = 0

    A_row = p.sb("A_row", [128, D], F32)
    B_row = p.sb("B_row", [128, D], F32)
    t0 = p.sb("t0", [128, D], F32)
    ident = p.sb("ident", [128, 128], BF16)
    wb = p.sb("wb", [128, 8, PW], BF16)
    stage = [p.sb(f"stg{i}", [128, PW], F32) for i in range(2)]
    xs = [p.sb(f"xs{i}", [128, D], F32) for i in range(2)]
    scr = dict(sq=p.sb("sq", [128, D], F32), ss=p.sb("ss", [128, 1], F32), rs=p.sb("rs", [128, 1], F32),
               hf=p.sb("hf", [128, D], F32), hb=p.sb("hb", [128, D], BF16))
    hT = [p.sb(f"hT{i}", [128, 8, 128], BF16) for i in range(2)]
    pf = [p.sb(f"pf{i}", [128, PW], F32) for i in range(2)]
    pb = [p.sb(f"pb{i}", [128, PW], BF16) for i in range(2)]
    s2 = p.sb("s2", [128, 64], F32)
    ss2 = p.sb("ss2", [128, 1], F32)
    tp_ps = p.ps("tp_ps", [128, 8, 128], BF16)
    mm_ps = [p.ps(f"mm_ps{i}", [128, 512], F32) for i in range(3)]

    p.dma("sp", ident[:], idb[:, :], reads=[idb], writes=[ident])
    p.dma("sp", A_row[:], nrm[:, :], reads=[nrm], writes=[A_row])
    p.dma("sp", t0[:], scr_[:, :], reads=[scr_], writes=[t0])
    p.dma("sp", B_row[:], shr_[:, :], reads=[shr_], writes=[B_row])
    p.op("dve", lambda e: e.scalar_tensor_tensor(out=A_row[:], in0=t0[:], scalar=1.0, in1=A_row[:], op0=ALU.add, op1=ALU.mult),
         reads=[t0, A_row], writes=[A_row])
    p.dma("sp", xs[0][:], x[0:128, :], reads=[x], writes=[xs[0]])
    load_w_bf16(p, w, wb, PW, stage)
    groups = [(g * 512, min(512, PW - g * 512)) for g in range(7)]
    gi = 0
    for c in range(CPC):
        xc = xs[c % 2]
        if c + 1 < CPC:
            p.dma("sp", xs[(c + 1) % 2][:], x[(c + 1) * 128:(c + 2) * 128, :], reads=[x], writes=[xs[(c + 1) % 2]])
        h = hT[c % 2]
        emit_norm_hT(p, xc, A_row, B_row, h, scr, ident, tp_ps, "a")
        f = pf[c % 2]
        bb = pb[c % 2]
        for (o0, n) in groups:
            ps = mm_ps[gi % 3]
            gi += 1
            for k in range(8):
                p.mm(ps, ps[:, 0:n], h, h[:, k, :], wb, wb[:, k, o0:o0 + n], start=(k == 0), stop=(k == 7))
            eng = "act" if gi % 2 == 0 else "dve"
            if eng == "act":
                p.op("act", lambda e: e.activation(out=f[:, o0:o0 + n], in_=ps[:, 0:n], func=AF.Copy), reads=[ps], writes=[f])
            else:
                p.op("dve", lambda e: e.tensor_copy(out=f[:, o0:o0 + n], in_=ps[:, 0:n]), reads=[ps], writes=[f])
        p.op("dve", lambda e: e.tensor_tensor(out=s2[:], in0=f[:, 3328:3392], in1=f[:, 3328:3392], op=ALU.mult), reads=[f], writes=[s2])
        p.op("dve", lambda e: e.tensor_reduce(out=ss2[:], in_=s2[:], axis=AX.X, op=ALU.add), reads=[s2], writes=[ss2])
        p.op("dve", lambda e: e.tensor_scalar(out=ss2[:], in0=ss2[:], scalar1=1.0 / 64, scalar2=EPS, op0=ALU.mult, op1=ALU.add), reads=[ss2], writes=[ss2])
        p.op("act", lambda e: e.activation(out=ss2[:], in_=ss2[:], func=AF.Sqrt), reads=[ss2], writes=[ss2])
        p.op("dve", lambda e: e.reciprocal(out=ss2[:], in_=ss2[:]), reads=[ss2], writes=[ss2])
        p.op("dve", lambda e: e.tensor_scalar(out=f[:, 3328:3392], in0=f[:, 3328:3392], scalar1=ss2[:, 0:1], scalar2=None, op0=ALU.mult), reads=[f, ss2], writes=[f])
        p.op("dve", lambda e: e.tensor_scalar(out=f[:, 3392:3396], in0=f[:, 3392:3396], scalar1=0.5, scalar2=None, op0=ALU.mult), reads=[f], writes=[f])
        p.op("pool", lambda e: e.tensor_copy(out=bb[:], in_=f[:]), reads=[f], writes=[bb])
        if fused is None:
            p.dma("sp", of[c * 128:(c + 1) * 128, :], f[:], reads=[f], writes=[of])
            p.dma("poolq", ob[c * 128:(c + 1) * 128, :], bb[:], reads=[bb], writes=[ob])
        else:
            cs_ = slice(c * 128, (c + 1) * 128)
            for (c0, n, dst) in ((0, 4, fused["qTm"]), (1536, 4, fused["qTd"]), (3072, 2, fused["iqT"])):
                ts_ = tsb[tsi % 2]
                tsi += 1
                for i in range(n):
                    p.op("pe", lambda e: e.transpose(out=tpf_ps[:, i * 128:(i + 1) * 128], in_=f[:, c0 + i * 128:c0 + (i + 1) * 128], identity=identf[:]),
                         reads=[f, identf], writes=[tpf_ps])
                p.op("act", lambda e: e.activation(out=ts_[:, 0:n * 128], in_=tpf_ps[:, 0:n * 128], func=AF.Copy), reads=[tpf_ps], writes=[ts_])
                dv = dst.h.rearrange("(i p) t -> p i t", p=128)[:, 0:n, cs_]
                p.dma("sp", dv, ts_[:, 0:n * 128].rearrange("p (i t) -> p i t", t=128), reads=[ts_], writes=[dst])
            ts_ = tsb[tsi % 2]
            tsi += 1
            p.op("pe", lambda e: e.transpose(out=tpf_ps[0:64, 0:128], in_=f[:, 3328:3392], identity=identf[:]), reads=[f, identf], writes=[tpf_ps])
            p.op("act", lambda e: e.activation(out=ts_[0:64, 0:128], in_=tpf_ps[0:64, 0:128], func=AF.Copy), reads=[tpf_ps], writes=[ts_])
            p.dma("sp", fused["ik_loc"].h.rearrange("(d j) t -> d j t", j=CPC)[:, c, :], ts_[0:64, 0:128], reads=[ts_], writes=[fused["ik_loc"]])
            for (c0, dst) in ((512, fused["kTm_loc"]), (2048, fused["kTd_loc"])):
                ks_ = ksb[ksi % 2]
                ksi += 1
                for i in range(4):
                    p.op("pe", lambda e: e.transpose(out=tpk_ps[:, i * 128:(i + 1) * 128], in_=bb[:, c0 + i * 128:c0 + (i + 1) * 128], identity=ident[:]),
                         reads=[bb, ident], writes=[tpk_ps])
                p.op("act", lambda e: e.activation(out=ks_[:], in_=tpk_ps[:, 0:512], func=AF.Copy), reads=[tpk_ps], writes=[ks_])
                dv = dst.h.rearrange("(i p j) t -> p i j t", p=128, j=CPC)[:, :, c, :]
                p.dma("sp", dv, ks_[:].rearrange("p (i t) -> p i t", t=128), reads=[ks_], writes=[dst])
            for vi, (c0, dst) in enumerate(((1024, fused["vam_loc"]), (2560, fused["vad_loc"]))):
                va_ = vaug[vi]
                p.op("pool", lambda e: e.tensor_copy(out=va_[:, :, 0:64], in_=bb[:, c0:c0 + 512].rearrange("p (h d) -> p h d", d=64)), reads=[bb], writes=[va_])
                p.dma("sp", dst.h.rearrange("(h p j) e -> p h j e", p=128, j=CPC)[:, :, c, :], va_[:], reads=[va_], writes=[dst])
            p.dma("sp", fused["iwd"][:, c * 4:(c + 1) * 4], f[:, 3392:3396], reads=[f], writes=[fused["iwd"]])
    if not standalone:
        p.barrier()
        for (loc, allt) in (("kTm_loc", "kTm_all"), ("kTd_loc", "kTd_all"), ("vam_loc", "vam_all"), ("vad_loc", "vad_all"), ("ik_loc", "ik_all")):
            ins = p.nc.gpsimd.collective_compute("AllGather", ALU.bypass, replica_groups=[list(range(NCORES))],
                                                 ins=[fused[loc][:, :]], outs=[fused[allt][:, :]])
            ins.then_inc(fused["cc_sem"])
            fused["cc_n"][0] += 1
        for w in list(p.eng.values()) + [p.issuer["sp"]]:
            w["h"].wait_ge(fused["cc_sem"], fused["cc_n"][0])
        p.barrier()
        return None
    p.finish([of, ob])
    return nc


def tok_perm():
    idx = np.arange(S).reshape(CPC, NCORES, 128)
    return np.ascontiguousarray(idx.transpose(1, 0, 2)).reshape(NCORES, TPC)


def rep(v):
    return np.ascontiguousarray(np.broadcast_to(np.asarray(v, np.float32)[None, :], (128, v.shape[0])))


def run_A(x2d, nrm, sc, sh, w_in):
    perm = tok_perm()
    nc = build_A()
    idb = np.eye(128, dtype=np.float32).astype(NPBF)
    maps = [{"x": np.ascontiguousarray(x2d[perm[i]]), "nrm": rep(nrm), "sc": rep(sc), "sh": rep(sh),
             "w": np.ascontiguousarray(w_in), "idb": idb} for i in range(NCORES)]
    res = run_bass_kernel_spmd(nc, maps, core_ids=list(range(NCORES)))
    pf = np.empty((S, PW), np.float32)
    pb = np.empty((S, PW), NPBF)
    for i in range(NCORES):
        pf[perm[i]] = res.results[i]["of"]
        pb[perm[i]] = res.results[i]["ob"]
    return pf, pb


NDELTA = 34


def t5_bucket_np(dist):
    d = np.maximum(dist, 0)
    df = np.maximum(d, 1).astype(np.float32)
    large = 16 + (np.log(df / np.float32(16)) / np.float32(np.log(256.0)) * np.float32(16)).astype(np.int32)
    return np.where(d < 16, d, np.minimum(large, 31))


def toeplitz_tables(rel_bias_heads, core):
    H = rel_bias_heads.shape[0]
    p = np.arange(128)[:, None, None]
    dl = (np.arange(NDELTA) - 8 + core)[None, :, None]
    f = np.arange(128)[None, None, :]
    dist = 128 * dl + f - p
    bk = t5_bucket_np(dist)
    out = rel_bias_heads[:, bk]
    out = np.where((dist < 0)[None], np.float32(NEG), out)
    return np.ascontiguousarray(out).astype(NPBF)


def chunk_ids(core):
    return [8 * j + core for j in range(CPC)]


def build_B(p=None, fused=None):
    standalone = p is None
    if standalone:
        nc = bass.Bass("TRN2", target_bir_lowering=False)
        p = P(nc)
    H = 8
    qT = p.dram("qT", [H, 64, TPC], F32, "ExternalInput")
    kT = p.dram("kT", [H, 64, S], BF16, "ExternalInput")
    va = p.dram("va", [H, 128, 128 * 65], BF16, "ExternalInput")
    Eoh = p.dram("Eoh", [64, S], BF16, "ExternalInput")
    tz = p.dram("tz", [H, 128, NDELTA * 128], BF16, "ExternalInput")
    tab31 = p.dram("tab31", [128, H], F32, "ExternalInput")
    cFM = p.dram("cFM", [128, CPC * 64], F32, "ExternalInput")
    cSM = p.dram("cSM", [128, CPC * 64], F32, "ExternalInput")
    cSA = p.dram("cSA", [128, CPC * 64], F32, "ExternalInput")
    cFAR = p.dram("cFAR", [128, CPC * 64], F32, "ExternalInput")
    idb = p.dram("idb", [128, 128], BF16, "ExternalInput")
    idf = p.dram("idf", [128, 128], F32, "ExternalInput")
    yT = p.dram("yT", [H, 64, TPC], F32, "ExternalOutput")

    KA = [p.sb(f"KA{i}", [128, S], BF16) for i in range(2)]
    VA = [p.sb(f"VA{i}", [128, 128 * 65], BF16) for i in range(2)]
    TZ = [p.sb(f"TZ{i}", [128, NDELTA * 128], BF16) for i in range(2)]
    qs = [p.sb(f"qs{i}", [64, TPC], F32) for i in range(2)]
    QA = [p.sb(f"QA{i}", [128, TPC], BF16) for i in range(2)]
    km = p.sb("km", [64, 64], F32)
    gate = p.sb("gate", [128, CPC * 64], F32)
    m8 = p.sb("m8", [128, CPC * 8], F32)
    MBx = p.sb("MBx", [128, CPC * 128], F32)
    FM = p.sb("FM", [128, CPC * 64], F32)
    SM = p.sb("SM", [128, CPC * 64], F32)
    SA = p.sb("SA", [128, CPC * 64], F32)
    FAR = p.sb("FAR", [128, CPC * 64], F32)
    t31 = p.sb("t31", [128, H], F32)
    identb = p.sb("identb", [128, 128], BF16)
    identf = p.sb("identf", [128, 128], F32)
    onesf = p.sb("onesf", [128, 64], F32)
    PT = [p.sb(f"PT{i}", [128, 512], BF16) for i in range(3)]
    rz = p.sb("rz", [128, 512], F32)
    bc = p.sb("bc", [64, 512], F32)
    ysb = [p.sb(f"ysb{i}", [64, 512], F32) for i in range(2)]

    gate_ps = p.ps("gate_ps", [128, CPC * 64], F32)
    tp_ps = p.ps("tp_ps", [128, 512], F32)
    S_ps = [p.ps(f"S_ps{i}", [128, 512], F32) for i in range(3)]
    o_ps = p.ps("o_ps", [128, 512], F32)
    bc_ps = p.ps("bc_ps", [64, 512], F32)

    for (sbt, dr) in ((FM, cFM), (SM, cSM), (SA, cSA), (FAR, cFAR), (t31, tab31), (identb, idb), (identf, idf)):
        p.dma("sp", sbt[:], dr[:, :], reads=[dr], writes=[sbt])
    p.op("dve", lambda e: e.memset(onesf[:], 1.0), writes=[onesf])
    p.op("dve", lambda e: e.memset(MBx[:], 0.0), writes=[MBx])
    for i in range(2):
        p.dma("sp", KA[i][64:128, :], Eoh[:, :], reads=[Eoh], writes=[KA[i]])

    if fused is not None:
        kall = fused["kTm_all"].h.rearrange("(r hh d j) p -> d hh r (j p)", r=NCORES, hh=8, d=64)
        vall = fused["vam_all"].h.rearrange("(r hh p j) e -> p hh r (j e)", r=NCORES, hh=8, p=128)
        tsum = p.sb("tsum", [64, 128], F32)
    stf = (lambda kt: (kt % 8) * 16 + kt // 8) if fused is not None else (lambda kt: kt)

    def load_head(h):
        b = h % 2
        if fused is None:
            p.dma("sp", KA[b][0:64, :], kT[h, :, :], reads=[kT], writes=[KA[b]])
            p.dma("sp", VA[b][:], va[h, :, :], reads=[va], writes=[VA[b]])
        else:
            p.dma("sp", KA[b][0:64, :].rearrange("d (r x) -> d r x", r=NCORES), kall[:, h, :, :], reads=[kT], writes=[KA[b]])
            p.dma("sp", VA[b][:].rearrange("p (r x) -> p r x", r=NCORES), vall[:, h, :, :], reads=[va], writes=[VA[b]])
        p.dma("sp", TZ[b][:], tz[h, :, :], reads=[tz], writes=[TZ[b]])
        p.dma("sp", qs[b][:], qT[h, :, :], reads=[qT], writes=[qs[b]])

    load_head(0)
    sidx = 0
    for h in range(H):
        b = h % 2
        if h + 1 < H:
            load_head(h + 1)
        ka, vab, tzb, qsb, qa = KA[b], VA[b], TZ[b], qs[b], QA[b]
        if fused is None:
            p.op("dve", lambda e: e.tensor_reduce(out=km[:], in_=ka[0:64, :].rearrange("p (j s) -> p j s", s=256), axis=AX.X, op=ALU.add),
                 reads=[ka], writes=[km])
        else:
            p.op("dve", lambda e: e.tensor_reduce(out=tsum[:], in_=ka[0:64, :].rearrange("p (j s) -> p j s", s=128), axis=AX.X, op=ALU.add),
                 reads=[ka], writes=[tsum])
            t4 = tsum[:].rearrange("d (a b j) -> d a b j", b=2, j=16)
            p.op("dve", lambda e: e.tensor_tensor(out=km[:].rearrange("d (a j) -> d a j", j=16), in0=t4[:, :, 0, :], in1=t4[:, :, 1, :], op=ALU.add),
                 reads=[tsum], writes=[km])
        p.op("act", lambda e: e.activation(out=qa[0:64, :], in_=qsb[:], func=AF.Copy, scale=0.125), reads=[qsb], writes=[qa])
        for j in range(CPC):
            p.mm(gate_ps, gate_ps[:, j * 64:(j + 1) * 64], qsb, qsb[:, j * 128:(j + 1) * 128], km, km[:, :], start=True, stop=True)
        p.op("dve", lambda e: e.tensor_tensor(out=gate[:], in0=gate_ps[:], in1=FM[:], op=ALU.add), reads=[gate_ps, FM], writes=[gate])
        for j in range(CPC):
            p.op("dve", lambda e: e.max(out=m8[:, j * 8:(j + 1) * 8], in_=gate[:, j * 64:(j + 1) * 64]), reads=[gate], writes=[m8])
        g3 = gate[:].rearrange("p (j n) -> p j n", n=64)
        th = m8[:].rearrange("p (j n) -> p j n", n=8)[:, :, 2:3].to_broadcast([128, CPC, 64])
        mb3 = MBx[:].rearrange("p (j n) -> p j n", n=128)[:, :, 64:128]
        p.op("dve", lambda e: e.tensor_tensor(out=mb3, in0=g3, in1=th, op=ALU.is_ge), reads=[gate, m8], writes=[MBx])
        p.op("dve", lambda e: e.tensor_scalar(out=mb3, in0=mb3, scalar1=-NEG, scalar2=NEG, op0=ALU.mult, op1=ALU.add), reads=[MBx], writes=[MBx])
        p.op("dve", lambda e: e.tensor_tensor(out=mb3, in0=mb3, in1=SM[:].rearrange("p (j n) -> p j n", n=64), op=ALU.mult), reads=[MBx, SM], writes=[MBx])
        p.op("dve", lambda e: e.tensor_tensor(out=mb3, in0=mb3, in1=SA[:].rearrange("p (j n) -> p j n", n=64), op=ALU.add), reads=[MBx, SA], writes=[MBx])
        p.op("dve", lambda e: e.scalar_tensor_tensor(out=mb3, in0=FAR[:].rearrange("p (j n) -> p j n", n=64), scalar=t31[:, h:h + 1], in1=mb3, op0=ALU.mult, op1=ALU.add),
             reads=[MBx, FAR, t31], writes=[MBx])
        for g in range(CPC // 4):
            for i in range(4):
                j = 4 * g + i
                p.op("pe", lambda e: e.transpose(out=tp_ps[:, i * 128:(i + 1) * 128], in_=MBx[:, j * 128:(j + 1) * 128], identity=identf[:]),
                     reads=[MBx, identf], writes=[tp_ps])
            p.op("act", lambda e: e.activation(out=qa[64:128, g * 512:(g + 1) * 512], in_=tp_ps[64:128, :], func=AF.Copy), reads=[tp_ps], writes=[qa])
        for g in range(CPC // 4):
            cs = [8 * (4 * g + i) + 7 for i in range(4)]
            cmax = cs[3]
            nkt = cmax + 1
            prev = None
            for kt in range(nkt):
                ps = S_ps[sidx % 3]
                pt = PT[sidx % 3]
                sidx += 1
                st = stf(kt)
                mms = [(ps[:, :], ka, ka[:, st * 128:(st + 1) * 128], qa, qa[:, g * 512:(g + 1) * 512])]
                for i in range(4):
                    dl = cs[i] - kt
                    if dl >= -1 and cs[i] - (2 * (kt // 2) + 1) <= 31:
                        di = dl + 1
                        mms.append((ps[:, i * 128:(i + 1) * 128], identb, identb[:, :], tzb, tzb[:, di * 128:(di + 1) * 128]))
                for n_, (o_ap, lt, l_ap, rt, r_ap) in enumerate(mms):
                    p.mm(ps, o_ap, lt, l_ap, rt, r_ap, start=(n_ == 0), stop=(n_ == len(mms) - 1))
                p.op("act", lambda e: e.activation(out=pt[:], in_=ps[:], func=AF.Exp), reads=[ps], writes=[pt])
                if prev is not None:
                    pkt, ppt = prev
                    p.mm(o_ps, o_ps[0:65, :], vab, vab[:, stf(pkt) * 65:stf(pkt) * 65 + 65], ppt, ppt[:, :], start=(pkt == 0), stop=False)
                prev = (kt, pt)
            pkt, ppt = prev
            p.mm(o_ps, o_ps[0:65, :], vab, vab[:, stf(pkt) * 65:stf(pkt) * 65 + 65], ppt, ppt[:, :], start=(pkt == 0), stop=True)
            p.op("dve", lambda e: e.reciprocal(out=rz[64:65, :], in_=o_ps[64:65, :]), reads=[o_ps], writes=[rz])
            p.mm(bc_ps, bc_ps[:, :], onesf, onesf[64:65, 0:64], rz, rz[64:65, :], start=True, stop=True)
            p.op("act", lambda e: e.activation(out=bc[:], in_=bc_ps[:], func=AF.Copy), reads=[bc_ps], writes=[bc])
            yb = ysb[g % 2]
            p.op("dve", lambda e: e.tensor_tensor(out=yb[:], in0=o_ps[0:64, :], in1=bc[:], op=ALU.mult), reads=[o_ps, bc], writes=[yb])
            p.dma("sp", yT[h, :, g * 512:(g + 1) * 512], yb[:], reads=[yb], writes=[yT])
    if not standalone:
        p.barrier()
        return None
    p.finish()
    return nc


def moba_consts(core):
    FM = np.zeros((CPC, 64), np.float32); SM = np.zeros((CPC, 64), np.float32)
    SA = np.zeros((CPC, 64), np.float32); FAR = np.zeros((CPC, 64), np.float32)
    blk = np.arange(64)
    for j in range(CPC):
        c = 8 * j + core
        b0 = c // 2
        FM[j] = np.where(blk >= b0, -1e30, 0.0)
        SM[j] = np.where(blk < b0, 1.0, 0.0)
        SA[j] = np.where(blk <= b0, 0.0, NEG)
        c7 = 8 * j + 7
        FAR[j] = np.where((c7 - (2 * blk + 1) > 31), 1.0, 0.0)
    r = lambda a: np.ascontiguousarray(np.broadcast_to(a.reshape(1, -1), (128, CPC * 64)))
    return r(FM), r(SM), r(SA), r(FAR)


def run_B(pf, pb, rel_bias):
    perm = tok_perm()
    nc = build_B()
    H = 8
    kT = np.ascontiguousarray(pb[:, 512:1024].reshape(S, H, 64).transpose(1, 2, 0))
    v = pb[:, 1024:1536].reshape(128, 128, H, 64)
    va = np.ones((H, 128, 128, 65), NPBF)
    va[:, :, :, 0:64] = v.transpose(2, 1, 0, 3)
    va = va.reshape(H, 128, 128 * 65)
    Eoh = (np.arange(S)[None, :] // 256 == np.arange(64)[:, None]).astype(np.float32).astype(NPBF)
    idb = np.eye(128, dtype=np.float32).astype(NPBF)
    idf = np.eye(128, dtype=np.float32)
    tab31 = np.ascontiguousarray(np.broadcast_to(rel_bias[None, 0:8, 31], (128, 8))).astype(np.float32)
    maps = []
    for i in range(NCORES):
        qT = np.ascontiguousarray(pf[perm[i], 0:512].reshape(TPC, H, 64).transpose(1, 2, 0))
        FM, SM, SA, FAR = moba_consts(i)
        tzt = toeplitz_tables(rel_bias[0:8], i).reshape(H, 128, NDELTA * 128)
        maps.append({"qT": qT, "kT": kT, "va": va, "Eoh": Eoh, "tz": tzt, "tab31": tab31,
                     "cFM": FM, "cSM": SM, "cSA": SA, "cFAR": FAR, "idb": idb, "idf": idf})
    res = run_bass_kernel_spmd(nc, maps, core_ids=list(range(NCORES)))
    return [r["yT"] for r in res.results]


MARK = -1.0e30
FUT = -3.0e30
MT_OFF = [128 * 128 * sum(8 * jj + 8 for jj in range(j)) for j in range(CPC + 1)]


def build_C(phases=(1, 2), p=None, fused=None):
    standalone = p is None
    if standalone:
        nc = bass.Bass("TRN2", target_bir_lowering=False)
        p = P(nc)
    H = 8
    iqT = p.dram("iqT", [2, 128, TPC], F32, "ExternalInput")
    ikT = p.dram("ikT", [128, S], F32, "ExternalInput")
    iw = p.dram("iw", [128, CPC * 4], F32, "ExternalInput")
    cCM = p.dram("cCM", [128, 1024], F32, "ExternalInput")
    idb = p.dram("idb", [128, 128], BF16, "ExternalInput")
    qT = p.dram("qT", [H, 64, TPC], F32, "ExternalInput")
    kT = p.dram("kT", [H, 64, S], BF16, "ExternalInput")
    va = p.dram("va", [H, 128, 128 * 65], BF16, "ExternalInput")
    tz = p.dram("tz", [H, 128, NDELTA * 128], BF16, "ExternalInput")
    tab31 = p.dram("tab31", [128, H], F32, "ExternalInput")
    yT = p.dram("yT", [H, 64, TPC], F32, "ExternalOutput")
    MTW = MT_OFF[CPC] // 128
    if 1 in phases and 2 in phases:
        mT_d = p.dram("mT_d", [128, MTW], BF16, "Internal")
    elif 1 in phases:
        mT_d = p.dram("mT_o", [128, MTW], BF16, "ExternalOutput")
    else:
        mT_d = p.dram("mT_i", [128, MTW], BF16, "ExternalInput")
    mT_d.track = True

    identb = p.sb("identb", [128, 128], BF16)
    p.dma("sp", identb[:], idb[:, :], reads=[idb], writes=[identb])
    stf = (lambda kt: (kt % 8) * 16 + kt // 8) if fused is not None else (lambda kt: kt)

    if 1 in phases:
        p.scope_begin()
        IK = p.sb("IK", [128, S], F32)
        I = p.sb("I", [128, S], F32)
        IQ = [p.sb(f"IQ{i}", [128, TPC], F32) for i in range(2)]
        W = p.sb("W", [128, CPC * 4], F32)
        CM = p.sb("CM", [128, 1024], F32)
        rl = [p.sb(f"rl{i}", [128, 512], F32) for i in range(3)]
        m8 = p.sb("m8", [128, 8], F32)
        mk = [p.sb(f"mk{i}", [128, 1024], BF16) for i in range(2)]
        mTs = [p.sb(f"mTs{i}", [128, 1024], BF16) for i in range(2)]
        I_ps = [p.ps(f"I_ps{i}", [128, 512], F32) for i in range(3)]
        tpm = [p.ps(f"tpm{i}", [128, 1024], BF16) for i in range(2)]
        if fused is None:
            p.dma("sp", IK[:], ikT[:, :], reads=[ikT], writes=[IK])
        else:
            ikall = fused["ik_all"].h.rearrange("(r d j) p -> d r (j p)", r=NCORES, d=64)
            for hb_ in (0, 64):
                p.dma("sp", IK[hb_:hb_ + 64, :].rearrange("d (r x) -> d r x", r=NCORES), ikall, reads=[ikT], writes=[IK])
        IK4 = IK[:].rearrange("d (r j p) -> d r j p", r=8, j=CPC)
        for i in range(2):
            p.dma("sp", IQ[i][:], iqT[i, :, :], reads=[iqT], writes=[IQ[i]])
        p.dma("sp", W[:], iw[:, :], reads=[iw], writes=[W])
        p.dma("sp", CM[:], cCM[:, :], reads=[cCM], writes=[CM])
        CM01 = p.sb("CM01", [128, 1024], BF16)
        p.op("pool", lambda e: e.tensor_scalar(out=CM01[:], in0=CM[:], scalar1=0.0, scalar2=None, op0=ALU.is_equal), reads=[CM], writes=[CM01])
        x = 0
        y = 0
        for j in range(CPC):
            N7 = 8 * j + 8
            ncols = N7 * 128
            for grp in range(N7 // 4):
                gs = slice(grp * 512, (grp + 1) * 512)
                for h in range(4):
                    ps = I_ps[x % 3]
                    r = rl[x % 3]
                    x += 1
                    hb = (h % 2) * 64
                    iq = IQ[h // 2]
                    if fused is None:
                        rhs_ap = IK[hb:hb + 64, gs]
                    else:
                        kt0 = 4 * grp
                        rhs_ap = IK4[hb:hb + 64, kt0 % 8:kt0 % 8 + 4, kt0 // 8, :]
                    p.mm(ps, ps[:, :], iq, iq[hb:hb + 64, j * 128:(j + 1) * 128], IK, rhs_ap, start=True, stop=True)
                    p.op("act", lambda e: e.activation(out=r[:], in_=ps[:], func=AF.Relu), reads=[ps], writes=[r])
                    wcol = W[:, j * 4 + h:j * 4 + h + 1]
                    if h == 0:
                        p.op("dve", lambda e: e.tensor_scalar(out=I[:, gs], in0=r[:], scalar1=wcol, scalar2=None, op0=ALU.mult),
                             reads=[r, W], writes=[I])
                    else:
                        p.op("dve", lambda e: e.scalar_tensor_tensor(out=I[:, gs], in0=r[:], scalar=wcol, in1=I[:, gs], op0=ALU.mult, op1=ALU.add),
                             reads=[r, W, I], writes=[I])
            p.op("pool", lambda e: e.tensor_tensor(out=I[:, ncols - 1024:ncols], in0=I[:, ncols - 1024:ncols], in1=CM[:], op=ALU.add),
                 reads=[I, CM], writes=[I])
            for rnd in range(min(32, ncols // 8)):
                p.op("dve", lambda e: e.max(out=m8[:], in_=I[:, 0:ncols]), reads=[I], writes=[m8])
                p.op("dve", lambda e: e.match_replace(out=I[:, 0:ncols], in_to_replace=m8[:], in_values=I[:, 0:ncols], imm_value=MARK),
                     reads=[I, m8], writes=[I])
            for pc in range(N7 // 8):
                m = mk[y % 2]
                mt = mTs[y % 2]
                tp = tpm[y % 2]
                y += 1
                p.op("pool", lambda e: e.tensor_scalar(out=m[:], in0=I[:, pc * 1024:(pc + 1) * 1024], scalar1=0.5 * MARK, scalar2=None, op0=ALU.is_le),
                     reads=[I], writes=[m])
                if pc == N7 // 8 - 1:
                    p.op("pool", lambda e: e.tensor_tensor(out=m[:], in0=m[:], in1=CM01[:], op=ALU.mult), reads=[m, CM01], writes=[m])
                for i in range(8):
                    p.op("pe", lambda e: e.transpose(out=tp[:, i * 128:(i + 1) * 128], in_=m[:, i * 128:(i + 1) * 128], identity=identb[:]),
                         reads=[m, identb], writes=[tp])
                p.op("act", lambda e: e.activation(out=mt[:], in_=tp[:], func=AF.Copy), reads=[tp], writes=[mt])
                o0 = MT_OFF[j] // 128 + pc * 1024
                p.dma("sp", mT_d[:, o0:o0 + 1024], mt[:], reads=[mt], writes=[mT_d])
        p.scope_end()

    if 2 in phases:
        p.scope_begin()
        KD = p.sb("KD", [64, S], BF16)
        VA = p.sb("VA", [128, 128 * 65], BF16)
        TZ = [p.sb(f"TZ{i}", [128, NDELTA * 128], BF16) for i in range(2)]
        qs = [p.sb(f"qs{i}", [64, TPC], F32) for i in range(2)]
        QD = [p.sb(f"QD{i}", [64, TPC], BF16) for i in range(2)]
        MT = [p.sb(f"MT{i}", [128, 128 * 128], BF16) for i in range(2)]
        t31 = p.sb("t31", [128, H], F32)
        onesf = p.sb("onesf", [128, 64], F32)
        PT = [p.sb(f"PT{i}", [128, 512], BF16) for i in range(5)]
        rz = p.sb("rz", [128, 512], F32)
        bc = p.sb("bc", [64, 512], F32)
        ysb = [p.sb(f"ysb{i}", [64, 512], F32) for i in range(2)]
        S_ps = [p.ps(f"S_ps{i}", [128, 512], F32) for i in range(5)]
        o_ps = [p.ps(f"o_ps{i}", [128, 512], F32) for i in range(2)]
        bc_ps = p.ps("bc_ps", [64, 512], F32)
        p.dma("sp", t31[:], tab31[:, :], reads=[tab31], writes=[t31])
        p.op("dve", lambda e: e.memset(onesf[:], 1.0), writes=[onesf])
        sidx = 0
        mi = 0

        def load_small(h):
            b = h % 2
            p.dma("sp", TZ[b][:], tz[h, :, :], reads=[tz], writes=[TZ[b]])
            p.dma("sp", qs[b][:], qT[h, :, :], reads=[qT], writes=[qs[b]])
        load_small(0)
        for h in range(H):
            b = h % 2
            if fused is None:
                p.dma("sp", KD[:], kT[h, :, :], reads=[kT], writes=[KD])
                p.dma("sp", VA[:], va[h, :, :], reads=[va], writes=[VA])
            else:
                kall = fused["kTd_all"].h.rearrange("(r hh d j) p -> d hh r (j p)", r=NCORES, hh=8, d=64)
                vall = fused["vad_all"].h.rearrange("(r hh p j) e -> p hh r (j e)", r=NCORES, hh=8, p=128)
                p.dma("sp", KD[:].rearrange("d (r x) -> d r x", r=NCORES), kall[:, h, :, :], reads=[kT], writes=[KD])
                p.dma("sp", VA[:].rearrange("p (r x) -> p r x", r=NCORES), vall[:, h, :, :], reads=[va], writes=[VA])
            if h + 1 < H:
                load_small(h + 1)
            tzb, qsb, qd = TZ[b], qs[b], QD[b]
            p.op("act", lambda e: e.activation(out=qd[:], in_=qsb[:], func=AF.Copy, scale=0.125), reads=[qsb], writes=[qd])
            for j in range(CPC):
                N7 = 8 * j + 8
                c7 = 8 * j + 7
                mt = MT[mi % 2]
                mi += 1
                o0 = MT_OFF[j] // 128
                p.dma("sp", mt[:, 0:N7 * 128], mT_d[:, o0:o0 + N7 * 128], reads=[mT_d], writes=[mt])
                ops = o_ps[(j // 4) % 2]
                ocol = slice((j % 4) * 128, (j % 4) * 128 + 128)
                prev = []
                for grp in range(N7 // 4):
                    ps = S_ps[sidx % 5]
                    pt = PT[sidx % 5]
                    sidx += 1
                    far = all((c7 - (4 * grp + i)) >= 32 for i in range(4))
                    mms = []
                    for i in range(4):
                        kt = 4 * grp + i
                        st = stf(kt)
                        mms.append((ps[:, i * 128:(i + 1) * 128], KD, KD[:, st * 128:(st + 1) * 128], qd, qd[:, j * 128:(j + 1) * 128], i == 0))
                    if not far:
                        for i in range(4):
                            kt = 4 * grp + i
                            di = min(c7 - kt + 1, NDELTA - 1)
                            mms.append((ps[:, i * 128:(i + 1) * 128], identb, identb[:, :], tzb, tzb[:, di * 128:(di + 1) * 128], False))
                    for n_, (o_ap, lt, l_ap, rt, r_ap, st) in enumerate(mms):
                        p.mm(ps, o_ap, lt, l_ap, rt, r_ap, start=st, stop=(n_ == len(mms) - 1))
                    if far:
                        p.op("act", lambda e: e.activation(out=pt[:], in_=ps[:], func=AF.Exp, bias=t31[:, h:h + 1]), reads=[ps, t31], writes=[pt])
                    else:
                        p.op("act", lambda e: e.activation(out=pt[:], in_=ps[:], func=AF.Exp), reads=[ps], writes=[pt])
                    eng = "pool" if grp % 4 == 3 else "dve"
                    p.op(eng, lambda e: e.tensor_tensor(out=pt[:], in0=pt[:], in1=mt[:, grp * 512:(grp + 1) * 512], op=ALU.mult),
                         reads=[pt, mt], writes=[pt])
                    prev.append((grp, pt))
                    while len(prev) > 3:
                        pgrp, ppt = prev.pop(0)
                        for i in range(4):
                            kt = 4 * pgrp + i
                            p.mm(ops, ops[0:65, ocol], VA, VA[:, stf(kt) * 65:stf(kt) * 65 + 65], ppt, ppt[:, i * 128:(i + 1) * 128], start=(kt == 0), stop=False)
                for (pgrp, ppt) in prev:
                    for i in range(4):
                        kt = 4 * pgrp + i
                        p.mm(ops, ops[0:65, ocol], VA, VA[:, stf(kt) * 65:stf(kt) * 65 + 65], ppt, ppt[:, i * 128:(i + 1) * 128], start=(kt == 0), stop=(kt == N7 - 1))
                if j % 4 == 3:
                    g = j // 4
                    p.op("dve", lambda e: e.reciprocal(out=rz[64:65, :], in_=ops[64:65, :]), reads=[ops], writes=[rz])
                    p.mm(bc_ps, bc_ps[:, :], onesf, onesf[64:65, 0:64], rz, rz[64:65, :], start=True, stop=True)
                    p.op("act", lambda e: e.activation(out=bc[:], in_=bc_ps[:], func=AF.Copy), reads=[bc_ps], writes=[bc])
                    yb = ysb[g % 2]
                    p.op("dve", lambda e: e.tensor_tensor(out=yb[:], in0=ops[0:64, :], in1=bc[:], op=ALU.mult), reads=[ops, bc], writes=[yb])
                    p.dma("sp", yT[h, :, g * 512:(g + 1) * 512], yb[:], reads=[yb], writes=[yT])
        p.scope_end()
    if not standalone:
        p.barrier()
        return None
    p.finish()
    return nc


def dsa_consts(core):
    CM = np.zeros((128, 8, 128), np.float32)
    q = np.arange(128)[:, None]
    s = np.arange(128)[None, :]
    for i in range(8):
        if i > core:
            CM[:, i, :] = FUT
        elif i == core:
            CM[:, i, :] = np.where(s > q, FUT, 0.0)
    return CM.reshape(128, 1024)


def run_C(pf, pb, rel_bias, phases=(1, 2), mT_in=None):
    perm = tok_perm()
    nc = build_C(phases)
    H = 8
    kT = np.ascontiguousarray(pb[:, 2048:2560].reshape(S, H, 64).transpose(1, 2, 0))
    v = pb[:, 2560:3072].reshape(128, 128, H, 64)
    va = np.ones((H, 128, 128, 65), NPBF)
    va[:, :, :, 0:64] = v.transpose(2, 1, 0, 3)
    va = va.reshape(H, 128, 128 * 65)
    ikT1 = np.ascontiguousarray(pf[:, 3328:3392].T)
    ikT = np.concatenate([ikT1, ikT1], axis=0)
    idb = np.eye(128, dtype=np.float32).astype(NPBF)
    tab31 = np.ascontiguousarray(np.broadcast_to(rel_bias[None, 8:16, 31], (128, 8))).astype(np.float32)
    maps = []
    for i in range(NCORES):
        rows = perm[i]
        qT = np.ascontiguousarray(pf[rows, 1536:2048].reshape(TPC, H, 64).transpose(1, 2, 0))
        iq = pf[rows, 3072:3328]
        iqT = np.ascontiguousarray(iq.T.reshape(2, 128, TPC))
        iw = np.ascontiguousarray(pf[rows, 3392:3396].reshape(CPC, 128, 4).transpose(1, 0, 2).reshape(128, CPC * 4))
        tzt = toeplitz_tables(rel_bias[8:16], i).reshape(H, 128, NDELTA * 128)
        m = {"iqT": iqT, "ikT": ikT, "iw": iw, "cCM": dsa_consts(i), "idb": idb, "qT": qT, "kT": kT, "va": va,
             "tz": tzt, "tab31": tab31}
        if mT_in is not None:
            m["mT_i"] = mT_in[i]
        maps.append(m)
    res = run_bass_kernel_spmd(nc, maps, core_ids=list(range(NCORES)))
    if 2 in phases:
        return [r["yT"] for r in res.results]
    return [r["mT_o"] for r in res.results]


def build_cast(R, C):
    nc = bass.Bass("TRN2", target_bir_lowering=False)
    p = P(nc)
    a = p.dram("a", [R, C], F32, "ExternalInput")
    o = p.dram("o", [R, C], BF16, "ExternalOutput")
    av = a.h.rearrange("(n p) c -> p n c", p=128)
    ov = o.h.rearrange("(n p) c -> p n c", p=128)
    n = R // 128
    st = [p.sb(f"st{i}", [128, C], F32) for i in range(3)]
    ob = [p.sb(f"ob{i}", [128, C], BF16) for i in range(3)]
    for i in range(n):
        s_, o_ = st[i % 3], ob[i % 3]
        p.dma("sp", s_[:], av[:, i, :], reads=[a], writes=[s_])
        eng = ("dve", "pool", "act")[i % 3]
        if eng == "act":
            p.op("act", lambda e: e.activation(out=o_[:], in_=s_[:], func=AF.Copy), reads=[s_], writes=[o_])
        else:
            p.op(eng, lambda e: e.tensor_copy(out=o_[:], in_=s_[:]), reads=[s_], writes=[o_])
        p.dma("poolq" if i % 2 else "sp", ov[:, i, :], o_[:], reads=[o_], writes=[o])
    p.finish()
    return nc


def run_cast(arr):
    shp = arr.shape
    arr = np.ascontiguousarray(arr).reshape(-1, 2048)
    R, C = arr.shape
    nc = build_cast(R // NCORES, C)
    maps = [{"a": np.ascontiguousarray(arr[i * (R // NCORES):(i + 1) * (R // NCORES)])} for i in range(NCORES)]
    res = run_bass_kernel_spmd(nc, maps, core_ids=list(range(NCORES)))
    return np.concatenate([r["o"] for r in res.results], axis=0).reshape(shp)


def build_D1(p=None):
    standalone = p is None
    if standalone:
        nc = bass.Bass("TRN2", target_bir_lowering=False)
        p = P(nc)
    x = p.dram("x", [TPC, D], F32, "ExternalInput")
    yT = p.dram("yT", [D, TPC], F32, "ExternalInput")
    wo = p.dram("wo", [D, D], F32, "ExternalInput")
    wq = p.dram("wq", [D, 2048], F32, "ExternalInput")
    skT = p.dram("skT", [128, 2048], F32, "ExternalInput")
    rg1 = p.dram("rg1", [128, D], F32, "ExternalInput")
    rnrm = p.dram("rnrm", [128, D], F32, "ExternalInput")
    rsc = p.dram("rsc", [128, D], F32, "ExternalInput")
    rsh = p.dram("rsh", [128, D], F32, "ExternalInput")
    idb = p.dram("idb", [128, 128], BF16, "ExternalInput")
    x1o = p.dram("x1o", [TPC, D], F32, "ExternalOutput")
    h2o = p.dram("h2o", [128, CPC * 1024], BF16, "ExternalOutput")
    so = p.dram("so", [TPC, 2048], F32, "ExternalOutput")

    ident = p.sb("ident", [128, 128], BF16)
    g1 = p.sb("g1", [128, D], F32)
    A2 = p.sb("A2", [128, D], F32)
    B2 = p.sb("B2", [128, D], F32)
    wob = p.sb("wob", [128, 8, D], BF16)
    wqb = p.sb("wqb", [128, 8, 2048], BF16)
    sk = p.sb("sk", [128, 2048], F32)
    p.dma("sp", ident[:], idb[:, :], reads=[idb], writes=[ident])
    p.dma("sp", g1[:], rg1[:, :], reads=[rg1], writes=[g1])
    p.dma("sp", A2[:], rnrm[:, :], reads=[rnrm], writes=[A2])
    p.dma("sp", B2[:], rsh[:, :], reads=[rsh], writes=[B2])
    p.dma("sp", sk[:], skT[:, :], reads=[skT], writes=[sk])
    p.scope_begin()
    t0 = p.sb("t0", [128, D], F32)
    stage = [p.sb(f"stg{i}", [128, 2048], F32) for i in range(2)]
    p.dma("sp", t0[:], rsc[:, :], reads=[rsc], writes=[t0])
    p.op("dve", lambda e: e.scalar_tensor_tensor(out=A2[:], in0=t0[:], scalar=1.0, in1=A2[:], op0=ALU.add, op1=ALU.mult),
         reads=[t0, A2], writes=[A2])
    load_w_bf16(p, wo, wob, D, stage)
    load_w_bf16(p, wq, wqb, 2048, stage)
    p.scope_end()

    xs = [p.sb(f"xs{i}", [128, D], F32) for i in range(2)]
    yf = [p.sb(f"yf{i}", [128, 8, 128], F32) for i in range(2)]
    yb = [p.sb(f"yb{i}", [128, 8, 128], BF16) for i in range(2)]
    x1 = [p.sb(f"x1{i}", [128, D], F32) for i in range(2)]
    tmp = p.sb("tmp", [128, D], F32)
    scr = dict(sq=p.sb("sq", [128, D], F32), ss=p.sb("ss", [128, 1], F32), rs=p.sb("rs", [128, 1], F32),
               hf=p.sb("hf", [128, D], F32), hb=p.sb("hb", [128, D], BF16))
    hT = [p.sb(f"hT{i}", [128, 8, 128], BF16) for i in range(2)]
    qTs = p.sb("qTs", [128, 16, 128], F32)
    ssb = [p.sb(f"ssb{i}", [128, 2048], F32) for i in range(2)]
    tp_ps = p.ps("tp_ps", [128, 8, 128], BF16)
    pA = [p.ps(f"pA{i}", [128, 512], F32) for i in range(4)]
    yTv = yT.h.rearrange("(k p) t -> p k t", p=128)

    def load_chunk(c):
        p.dma("sp", xs[c % 2][:], x[c * 128:(c + 1) * 128, :], reads=[x], writes=[xs[c % 2]])
        p.dma("sp", yf[c % 2][:], yTv[:, :, c * 128:(c + 1) * 128], reads=[yT], writes=[yf[c % 2]])
    load_chunk(0)
    pi = 0
    for c in range(CPC):
        if c + 1 < CPC:
            load_chunk(c + 1)
        xc, yfc, ybc, x1c, h = xs[c % 2], yf[c % 2], yb[c % 2], x1[c % 2], hT[c % 2]
        p.op("pool", lambda e: e.tensor_copy(out=ybc[:], in_=yfc[:]), reads=[yfc], writes=[ybc])
        for half in range(2):
            ps = pA[pi % 4]
            pi += 1
            hs = slice(half * 512, (half + 1) * 512)
            for k in range(8):
                p.mm(ps, ps[:, :], ybc, ybc[:, k, :], wob, wob[:, k, hs], start=(k == 0), stop=(k == 7))
            p.op("dve", lambda e: e.tensor_tensor(out=tmp[:, hs], in0=ps[:, :], in1=g1[:, hs], op=ALU.mult), reads=[ps, g1], writes=[tmp])
        p.op("pool", lambda e: e.tensor_tensor(out=x1c[:], in0=xc[:], in1=tmp[:], op=ALU.add), reads=[xc, tmp], writes=[x1c])
        p.dma("poolq", x1o[c * 128:(c + 1) * 128, :], x1c[:], reads=[x1c], writes=[x1o])
        emit_norm_hT(p, x1c, A2, B2, h, scr, ident, tp_ps, "d")
        p.dma("poolq", h2o[:, c * 1024:(c + 1) * 1024], h[:].rearrange("p k t -> p (k t)"), reads=[h], writes=[h2o])
        for g4 in range(4):
            ps = pA[pi % 4]
            pi += 1
            for i in range(4):
                hp = g4 * 4 + i
                for k in range(8):
                    p.mm(ps, ps[:, i * 128:(i + 1) * 128], wqb, wqb[:, k, hp * 128:(hp + 1) * 128], h, h[:, k, :],
                         start=(i == 0 and k == 0), stop=(i == 3 and k == 7))
            p.op("act", lambda e: e.activation(out=qTs[:, g4 * 4:(g4 + 1) * 4, :], in_=ps[:, :].rearrange("p (i t) -> p i t", t=128), func=AF.Copy),
                 reads=[ps], writes=[qTs])
        sb_ = ssb[c % 2]
        for g4 in range(4):
            ps = pA[pi % 4]
            pi += 1
            for i in range(4):
                hp = g4 * 4 + i
                p.mm(ps, ps[:, i * 128:(i + 1) * 128], qTs, qTs[:, hp, :], sk, sk[:, hp * 128:(hp + 1) * 128], start=(i == 0), stop=(i == 3))
            p.op("dve", lambda e: e.tensor_copy(out=sb_[:, g4 * 512:(g4 + 1) * 512], in_=ps[:, :]), reads=[ps], writes=[sb_])
        p.dma("sp", so[c * 128:(c + 1) * 128, :], sb_[:], reads=[sb_], writes=[so])
    if not standalone:
        p.barrier()
        return None
    p.finish()
    return nc


def run_D1(x2d, yTs, w_out, wq, subkeys, g1, nrm, sc, sh):
    perm = tok_perm()
    nc = build_D1()
    idb = np.eye(128, dtype=np.float32).astype(NPBF)
    skT = np.ascontiguousarray(subkeys.reshape(16, 128, 128).transpose(2, 0, 1).reshape(128, 2048))
    maps = [{"x": np.ascontiguousarray(x2d[perm[i]]), "yT": np.ascontiguousarray(yTs[i]), "wo": np.ascontiguousarray(w_out),
             "wq": np.ascontiguousarray(wq), "skT": skT, "rg1": rep(g1), "rnrm": rep(nrm), "rsc": rep(sc), "rsh": rep(sh),
             "idb": idb} for i in range(NCORES)]
    res = run_bass_kernel_spmd(nc, maps, core_ids=list(range(NCORES)))
    return [(r["x1o"], r["h2o"], r["so"]) for r in res.results]


def build_D2(final, p=None):
    standalone = p is None
    if standalone:
        nc = bass.Bass("TRN2", target_bir_lowering=False)
        p = P(nc)
    x1d = p.dram("x1", [TPC, D], F32, "ExternalInput")
    h2d = p.dram("h2T", [128, CPC * 1024], BF16, "ExternalInput")
    sd = p.dram("s", [TPC, 2048], F32, "ExternalInput")
    uTd = p.dram("uT", [D, 16384], BF16, "ExternalInput")
    vd = p.dram("v", [16384, D], BF16, "ExternalInput")
    rg2 = p.dram("rg2", [128, D], F32, "ExternalInput")
    rnf = p.dram("rnf", [128, D], F32, "ExternalInput")
    idb = p.dram("idb", [128, 128], BF16, "ExternalInput")
    xo = p.dram("xo", [TPC, D], F32, "ExternalOutput")

    ident = p.sb("ident", [128, 128], BF16)
    g2 = p.sb("g2", [128, D], F32)
    nf = p.sb("nf", [128, D], F32)
    p.dma("sp", ident[:], idb[:, :], reads=[idb], writes=[ident])
    p.dma("sp", g2[:], rg2[:, :], reads=[rg2], writes=[g2])
    p.dma("sp", nf[:], rnf[:, :], reads=[rnf], writes=[nf])
    G = [p.sb(f"G{i}", [128, 16384], BF16) for i in range(2)]
    H2 = p.sb("H2", [128, 8, 256], BF16)
    x1t = [p.sb(f"x1t{i}", [128, D], F32) for i in range(2)]
    out_ps = [p.ps(f"out_ps{i}", [128, 512], F32) for i in range(4)]
    pB = [p.ps(f"pB{i}", [128, 512], F32) for i in range(2)]
    tpb = [p.ps(f"tpb{i}", [128, 1024], BF16) for i in range(2)]
    uTv = uTd.h.rearrange("(k p) e -> p k e", p=128)
    vv = vd.h.rearrange("(i p) d -> p i d", p=128)
    h2v = h2d.h.rearrange("p (c k t) -> p c k t", k=8, t=128)

    for gi in range(CPC // 2):
        for cc in range(2):
            c = 2 * gi + cc
            p.dma("sp", H2[:, :, cc * 128:(cc + 1) * 128], h2v[:, c, :, :], reads=[h2d], writes=[H2])
            p.dma("sp", x1t[cc][:], x1d[c * 128:(c + 1) * 128, :], reads=[x1d], writes=[x1t[cc]])
        p.scope_begin()
        ssb = [p.sb(f"ssb{i}", [128, 2048], F32) for i in range(2)]
        sw = p.sb("sw", [128, 2048], F32)
        m16 = p.sb("m16", [128, 256], F32)
        cand = p.sb("cand", [128, 2048], F32)
        cw = p.sb("cw", [128, 2048], F32)
        g16 = p.sb("g16", [128, 128], F32)
        gm = p.sb("gm", [128, 128], F32)
        Z = p.sb("Z", [128, 8], F32)
        nb = p.sb("nb", [128, 8], F32)
        Bt = [p.sb(f"Bt{i}", [128, 2048], F32) for i in range(2)]
        Et = [p.sb(f"Et{i}", [128, 2048], BF16) for i in range(2)]
        Gh = [p.sb(f"Gh{i}", [128, 2048], BF16) for i in range(2)]
        for cc in range(2):
            c = 2 * gi + cc
            p.dma("sp", ssb[cc][:], sd[c * 128:(c + 1) * 128, :], reads=[sd], writes=[ssb[cc]])
        bi = 0
        for cc in range(2):
            sb_ = ssb[cc]
            s3 = sb_[:].rearrange("p (a n) -> p a n", n=128)
            sw3 = sw[:].rearrange("p (a n) -> p a n", n=128)
            m3 = m16[:].rearrange("p (a n) -> p a n", n=16)
            for hp in range(16):
                p.op("dve", lambda e: e.max(out=m3[:, hp, 0:8], in_=s3[:, hp, :]), reads=[sb_], writes=[m16])
                p.op("dve", lambda e: e.match_replace(out=sw3[:, hp, :], in_to_replace=m3[:, hp, 0:8], in_values=s3[:, hp, :], imm_value=MARK),
                     reads=[sb_, m16], writes=[sw])
                p.op("dve", lambda e: e.max(out=m3[:, hp, 8:16], in_=sw3[:, hp, :]), reads=[sw], writes=[m16])
            m4 = m16[:].rearrange("p (h t n) -> p h t n", t=2, n=16)
            c4 = cand[:].rearrange("p (h a b) -> p h a b", a=16, b=16)
            p.op("dve", lambda e: e.tensor_tensor(out=c4, in0=m4[:, :, 0, :].unsqueeze(3).to_broadcast([128, 8, 16, 16]),
                                                  in1=m4[:, :, 1, :].unsqueeze(2).to_broadcast([128, 8, 16, 16]), op=ALU.add),
                 reads=[m16], writes=[cand])
            c3 = cand[:].rearrange("p (h n) -> p h n", n=256)
            cw3 = cw[:].rearrange("p (h n) -> p h n", n=256)
            g3 = g16[:].rearrange("p (h n) -> p h n", n=16)
            for h in range(8):
                p.op("dve", lambda e: e.max(out=g3[:, h, 0:8], in_=c3[:, h, :]), reads=[cand], writes=[g16])
                p.op("dve", lambda e: e.match_replace(out=cw3[:, h, :], in_to_replace=g3[:, h, 0:8], in_values=c3[:, h, :], imm_value=MARK),
                     reads=[cand, g16], writes=[cw])
                p.op("dve", lambda e: e.max(out=g3[:, h, 8:16], in_=cw3[:, h, :]), reads=[cw], writes=[g16])
            gm3 = gm[:].rearrange("p (h n) -> p h n", n=16)
            p.op("dve", lambda e: e.tensor_tensor(out=gm3, in0=g3, in1=g3[:, :, 0:1].to_broadcast([128, 8, 16]), op=ALU.subtract),
                 reads=[g16], writes=[gm])
            p.op("act", lambda e: e.activation(out=gm[:], in_=gm[:], func=AF.Exp), reads=[gm], writes=[gm])
            p.op("dve", lambda e: e.tensor_reduce(out=Z[:], in_=gm3, axis=AX.X, op=ALU.add), reads=[gm], writes=[Z])
            p.op("act", lambda e: e.activation(out=Z[:], in_=Z[:], func=AF.Ln), reads=[Z], writes=[Z])
            p.op("dve", lambda e: e.scalar_tensor_tensor(out=nb[:], in0=g3[:, :, 0], scalar=-1.0, in1=Z[:], op0=ALU.mult, op1=ALU.subtract),
                 reads=[g16, Z], writes=[nb])
            Gc = G[cc]
            for h in range(8):
                for e8 in range(8):
                    B_, E_, Gh_ = Bt[bi % 2], Et[bi % 2], Gh[bi % 2]
                    bi += 1
                    cols = slice(e8 * 2048, (e8 + 1) * 2048)
                    in0 = s3[:, 2 * h, e8 * 16:(e8 + 1) * 16].unsqueeze(2).to_broadcast([128, 16, 128])
                    in1 = s3[:, 2 * h + 1, :].unsqueeze(1).to_broadcast([128, 16, 128])
                    B3 = B_[:].rearrange("p (i j) -> p i j", j=128)
                    p.op("pool", lambda e: e.tensor_tensor(out=B3, in0=in0, in1=in1, op=ALU.add), reads=[sb_], writes=[B_])
                    p.op("act", lambda e: e.activation(out=E_[:], in_=B_[:], func=AF.Exp, bias=nb[:, h:h + 1]), reads=[B_, nb], writes=[E_])
                    th = g3[:, h, 15:16]
                    if h == 0:
                        p.op("dve", lambda e: e.scalar_tensor_tensor(out=Gc[:, cols], in0=B_[:], scalar=th, in1=E_[:], op0=ALU.is_ge, op1=ALU.mult),
                             reads=[B_, E_, g16], writes=[Gc])
                    else:
                        p.op("dve", lambda e: e.scalar_tensor_tensor(out=Gh_[:], in0=B_[:], scalar=th, in1=E_[:], op0=ALU.is_ge, op1=ALU.mult),
                             reads=[B_, E_, g16], writes=[Gh_])
                        p.op("dve", lambda e: e.tensor_tensor(out=Gc[:, cols], in0=Gc[:, cols], in1=Gh_[:], op=ALU.add), reads=[Gc, Gh_], writes=[Gc])
        p.scope_end()
        p.scope_begin()
        ub = [p.sb(f"ub{i}", [128, 8, 512], BF16) for i in range(2)]
        vb = [p.sb(f"vb{i}", [128, 4, D], BF16) for i in range(2)]
        ga = [p.sb(f"ga{i}", [128, 256], BF16) for i in range(2)]
        GAs = [p.sb(f"GAs{i}", [128, 256], BF16) for i in range(2)]
        tmp = p.sb("tmp", [128, D], F32)
        x2 = p.sb("x2", [128, D], F32)
        sq = p.sb("sq", [128, D], F32)
        ss = p.sb("ss", [128, 1], F32)

        def load_uv(i4):
            b = i4 % 2
            p.dma("sp", ub[b][:], uTv[:, :, i4 * 512:(i4 + 1) * 512], reads=[uTd], writes=[ub[b]])
            p.dma("poolq", vb[b][:], vv[:, i4 * 4:(i4 + 1) * 4, :], reads=[vd], writes=[vb[b]])
        load_uv(0)

        def emit_out(it, GA_):
            i4, ii = it // 4, it % 4
            v_ = vb[i4 % 2]
            for cc in range(2):
                for half in range(2):
                    ops = out_ps[cc * 2 + half]
                    p.mm(ops, ops[:, :], GA_, GA_[:, cc * 128:(cc + 1) * 128], v_, v_[:, ii, half * 512:(half + 1) * 512],
                         start=(it == 0), stop=(it == 127))
        pend = None
        for it in range(128):
            i4, ii = it // 4, it % 4
            u_ = ub[i4 % 2]
            aps, gT, ga_, GA_ = pB[it % 2], tpb[it % 2], ga[it % 2], GAs[it % 2]
            if ii == 0 and i4 + 1 < 32 and pend is None:
                load_uv(i4 + 1)
            for k in range(8):
                p.mm(aps, aps[:, 0:256], u_, u_[:, k, ii * 128:(ii + 1) * 128], H2, H2[:, k, :], start=(k == 0), stop=(k == 7))
            p.op("act", lambda e: e.activation(out=ga_[:], in_=aps[:, 0:256], func=AF.Gelu), reads=[aps], writes=[ga_])
            for cc in range(2):
                p.op("pe", lambda e: e.transpose(out=gT[:, cc * 128:(cc + 1) * 128], in_=G[cc][:, it * 128:(it + 1) * 128], identity=ident[:]),
                     reads=[G[cc], ident], writes=[gT])
            if pend is not None:
                emit_out(*pend)
                if ii == 0 and i4 + 1 < 32:
                    load_uv(i4 + 1)
            p.op("dve", lambda e: e.tensor_tensor(out=GA_[:], in0=gT[:, 0:256], in1=ga_[:], op=ALU.mult), reads=[gT, ga_], writes=[GA_])
            pend = (it, GA_)
        emit_out(*pend)
        for cc in range(2):
            c = 2 * gi + cc
            for half in range(2):
                hs = slice(half * 512, (half + 1) * 512)
                p.op("dve", lambda e: e.tensor_tensor(out=tmp[:, hs], in0=out_ps[cc * 2 + half][:, :], in1=g2[:, hs], op=ALU.mult),
                     reads=[out_ps[cc * 2 + half], g2], writes=[tmp])
            p.op("pool", lambda e: e.tensor_tensor(out=x2[:], in0=x1t[cc][:], in1=tmp[:], op=ALU.add), reads=[x1t[cc], tmp], writes=[x2])
            if final:
                p.op("act", lambda e: e.activation(out=sq[:], in_=x2[:], func=AF.Square, accum_out=ss[:]), reads=[x2], writes=[sq, ss])
                p.op("dve", lambda e: e.tensor_scalar(out=ss[:], in0=ss[:], scalar1=1.0 / D, scalar2=EPS, op0=ALU.mult, op1=ALU.add), reads=[ss], writes=[ss])
                p.op("act", lambda e: e.activation(out=ss[:], in_=ss[:], func=AF.Sqrt), reads=[ss], writes=[ss])
                p.op("dve", lambda e: e.reciprocal(out=ss[:], in_=ss[:]), reads=[ss], writes=[ss])
                p.op("dve", lambda e: e.scalar_tensor_tensor(out=x2[:], in0=x2[:], scalar=ss[:, 0:1], in1=nf[:], op0=ALU.mult, op1=ALU.mult),
                     reads=[x2, ss, nf], writes=[x2])
            p.dma("sp", xo[c * 128:(c + 1) * 128, :], x2[:], reads=[x2], writes=[xo])
        p.scope_end()
    if not standalone:
        p.barrier()
        return None
    p.finish()
    return nc


def run_D2(d1res, uT_bf, v_bf, g2, nfin, final):
    nc = build_D2(final)
    idb = np.eye(128, dtype=np.float32).astype(NPBF)
    maps = [{"x1": d1res[i][0], "h2T": d1res[i][1], "s": d1res[i][2], "uT": uT_bf, "v": v_bf, "rg2": rep(g2), "rnf": rep(nfin),
             "idb": idb} for i in range(NCORES)]
    res = run_bass_kernel_spmd(nc, maps, core_ids=list(range(NCORES)))
    perm = tok_perm()
    out = np.empty((S, D), np.float32)
    for i in range(NCORES):
        out[perm[i]] = res.results[i]["xo"]
    return out


def emit_modrep(p, cT, w_ada, b_rows, modrep):
    p.scope_begin()
    cs = p.sb("cs", [128, 8], F32)
    sc = p.sb("sc", [128, 8], F32)
    screp = p.sb("screp", [128, 8, 128], F32)
    stg = [p.sb(f"wst{i}", [128, 8, 512], F32) for i in range(2)]
    brow = [p.sb(f"brow{i}", [128, 512], F32) for i in range(2)]
    osb = [p.sb(f"osb{i}", [128, 512], F32) for i in range(2)]
    acc = [p.ps(f"macc{i}", [128, 512], F32) for i in range(2)]
    p.dma("sp", cs[:], cT[:, :], writes=[cs])
    p.op("act", lambda e: e.activation(out=sc[:], in_=cs[:], func=AF.Silu), reads=[cs], writes=[sc])
    p.op("dve", lambda e: e.tensor_copy(out=screp[:], in_=sc[:].unsqueeze(2).to_broadcast([128, 8, 128])), reads=[sc], writes=[screp])
    gi = 0
    for l in range(2):
        wv = w_ada.h[l].rearrange("(k p) n -> p k n", p=128)
        for g in range(12):
            st, br, ob_, ac = stg[gi % 2], brow[gi % 2], osb[gi % 2], acc[gi % 2]
            gi += 1
            cols = slice(g * 512, (g + 1) * 512)
            p.dma("sp", st[:], wv[:, :, cols], writes=[st])
            p.dma("poolq", br[:], b_rows[:, l * 6144 + g * 512:l * 6144 + (g + 1) * 512], writes=[br])
            for k in range(8):
                p.mm(ac, ac[:, :], screp, screp[:, k, :], st, st[:, k, :], start=(k == 0), stop=(k == 7))
            p.op("dve", lambda e: e.tensor_tensor(out=ob_[:], in0=ac[:], in1=br[:], op=ALU.add), reads=[ac, br], writes=[ob_])
            p.dma("sp", modrep[:, l * 6144 + g * 512:l * 6144 + (g + 1) * 512], ob_[:], reads=[ob_], writes=[modrep])
    p.scope_end()


def emit_cast(p, src_ap, dst_ap, ntiles):
    p.scope_begin()
    st = [p.sb(f"cst{i}", [128, 2048], F32) for i in range(3)]
    ob = [p.sb(f"cob{i}", [128, 2048], BF16) for i in range(3)]
    for i in range(ntiles):
        s_, o_ = st[i % 3], ob[i % 3]
        p.dma("sp", s_[:], src_ap[:, i, :], writes=[s_])
        eng = ("dve", "pool", "act")[i % 3]
        if eng == "act":
            p.op("act", lambda e: e.activation(out=o_[:], in_=s_[:], func=AF.Copy), reads=[s_], writes=[o_])
        else:
            p.op(eng, lambda e: e.tensor_copy(out=o_[:], in_=s_[:]), reads=[s_], writes=[o_])
        p.dma("poolq" if i % 2 else "sp", dst_ap[:, i, :], o_[:], reads=[o_], writes=[])
    p.scope_end()


def build_fused():
    nc = bass.Bass("TRN2", target_bir_lowering=False)
    p = P(nc)
    EI, EO, IN = "ExternalInput", "ExternalOutput", "Internal"
    x = p.dram("x", [TPC, D], F32, EI)
    cT = p.dram("cT", [128, 8], F32, EI)
    w_ada = p.dram("w_ada", [2, D, 6144], F32, EI)
    b_rows = p.dram("b_rows", [128, 12288], F32, EI)
    rows = p.dram("rows", [5, 128, D], F32, EI)
    w_in = p.dram("w_in", [2, D, PW], F32, EI)
    w_out = p.dram("w_out", [2, D, D], F32, EI)
    wq = p.dram("wq", [2, D, 2048], F32, EI)
    skT = p.dram("skT", [2, 128, 2048], F32, EI)
    uT = p.dram("uT", [2, D, 16384], F32, EI)
    v = p.dram("v", [2, 16384, D], F32, EI)
    idb = p.dram("idb", [128, 128], BF16, EI)
    idf = p.dram("idf", [128, 128], F32, EI)
    Eoh = p.dram("Eoh", [64, S], BF16, EI)
    tzm = p.dram("tzm", [8, 128, NDELTA * 128], BF16, EI)
    tzd = p.dram("tzd", [8, 128, NDELTA * 128], BF16, EI)
    t31m = p.dram("t31m", [128, 8], F32, EI)
    t31d = p.dram("t31d", [128, 8], F32, EI)
    cFM = p.dram("cFM", [128, CPC * 64], F32, EI)
    cSM = p.dram("cSM", [128, CPC * 64], F32, EI)
    cSA = p.dram("cSA", [128, CPC * 64], F32, EI)
    cFAR = p.dram("cFAR", [128, CPC * 64], F32, EI)
    cCM = p.dram("cCM", [128, 1024], F32, EI)
    out = p.dram("out", [TPC, D], F32, EO)

    modrep = p.dram("modrep", [128, 12288], F32, IN)
    uTb = p.dram("uTb", [2, D, 16384], BF16, IN)
    vb = p.dram("vb", [2, 16384, D], BF16, IN)
    fz = dict(idf=idf,
              qTm=p.dram("qTm", [512, TPC], F32, IN), qTd=p.dram("qTd", [512, TPC], F32, IN),
              iqT=p.dram("iqT", [256, TPC], F32, IN), iwd=p.dram("iwd", [128, CPC * 4], F32, IN),
              kTm_loc=p.dram("kTm_loc", [CPC * 512, 128], BF16, IN), kTd_loc=p.dram("kTd_loc", [CPC * 512, 128], BF16, IN),
              vam_loc=p.dram("vam_loc", [8 * 128 * CPC, 65], BF16, IN), vad_loc=p.dram("vad_loc", [8 * 128 * CPC, 65], BF16, IN),
              ik_loc=p.dram("ik_loc", [CPC * 64, 128], F32, IN),
              kTm_all=p.dram("kTm_all", [NCORES * CPC * 512, 128], BF16, IN), kTd_all=p.dram("kTd_all", [NCORES * CPC * 512, 128], BF16, IN),
              vam_all=p.dram("vam_all", [NCORES * 8 * 128 * CPC, 65], BF16, IN), vad_all=p.dram("vad_all", [NCORES * 8 * 128 * CPC, 65], BF16, IN),
              ik_all=p.dram("ik_all", [NCORES * CPC * 64, 128], F32, IN),
              cc_sem=p._sem("cc_sem"), cc_n=[0])
    yT_all = p.dram("yT_all", [D, TPC], F32, IN)
    x1o = p.dram("x1o", [TPC, D], F32, IN)
    h2o = p.dram("h2o", [128, CPC * 1024], BF16, IN)
    so = p.dram("so", [TPC, 2048], F32, IN)
    xmid = p.dram("xmid", [TPC, D], F32, IN)

    V = lambda ap: T(ap, "view", track=False)
    emit_modrep(p, cT, w_ada, b_rows, modrep)
    for l in range(2):
        emit_cast(p, uT.h[l].rearrange("a (b c) -> (a b) c", c=2048).rearrange("(n q) c -> q n c", q=128),
                  uTb.h[l].rearrange("a (b c) -> (a b) c", c=2048).rearrange("(n q) c -> q n c", q=128), 64)
        emit_cast(p, v.h[l].rearrange("(a b) c -> a (b c)", b=2).rearrange("(n q) c -> q n c", q=128),
                  vb.h[l].rearrange("(a b) c -> a (b c)", b=2).rearrange("(n q) c -> q n c", q=128), 64)
    mcol = lambda l, i: V(modrep.h[:, l * 6144 + i * 1024:l * 6144 + (i + 1) * 1024])
    yv = yT_all.h.rearrange("(a h d) t -> a h d t", a=2, h=8)
    xin = x
    for l in range(2):
        xout = xmid if l == 0 else out
        p.bind = {"x": xin, "nrm": V(rows.h[l]), "sc": mcol(l, 1), "sh": mcol(l, 0), "w": V(w_in.h[l]), "idb": idb}
        p.scope_begin(); build_A(p=p, fused=fz); p.scope_end()
        p.bind = {"qT": V(fz["qTm"].h.rearrange("(h d) t -> h d t", d=64)), "kT": fz["kTm_all"], "va": fz["vam_all"], "Eoh": Eoh,
                  "tz": tzm, "tab31": t31m, "cFM": cFM, "cSM": cSM, "cSA": cSA, "cFAR": cFAR, "idb": idb, "idf": idf, "yT": V(yv[0])}
        p.scope_begin(); build_B(p=p, fused=fz); p.scope_end()
        p.bind = {"iqT": V(fz["iqT"].h.rearrange("(a q) t -> a q t", a=2)), "ikT": fz["ik_all"], "iw": fz["iwd"], "cCM": cCM, "idb": idb,
                  "qT": V(fz["qTd"].h.rearrange("(h d) t -> h d t", d=64)), "kT": fz["kTd_all"], "va": fz["vad_all"], "tz": tzd,
                  "tab31": t31d, "yT": V(yv[1])}
        p.scope_begin(); build_C(p=p, fused=fz); p.scope_end()
        p.bind = {"x": xin, "yT": yT_all, "wo": V(w_out.h[l]), "wq": V(wq.h[l]), "skT": V(skT.h[l]), "rg1": mcol(l, 2),
                  "rnrm": V(rows.h[2 + l]), "rsc": mcol(l, 4), "rsh": mcol(l, 3), "idb": idb, "x1o": x1o, "h2o": h2o, "so": so}
        p.scope_begin(); build_D1(p=p); p.scope_end()
        p.bind = {"x1": x1o, "h2T": h2o, "s": so, "uT": V(uTb.h[l]), "v": V(vb.h[l]), "rg2": mcol(l, 5), "rnf": V(rows.h[4]),
                  "idb": idb, "xo": xout}
        p.scope_begin(); build_D2(l == 1, p=p); p.scope_end()
        xin = xmid
    p.bind = {}
    p.finish()
    return nc


def perm_bs(a):
    bs = np.arange(64)
    b = 4 * (bs % 16) + bs // 16
    return a[:, b]


def kernel(x, c, w_ada, b_ada, norm_attn, norm_ffn, w_in, w_out, rel_bias,
           peer_wq, peer_subkeys, peer_u, peer_v, norm_final):
    f = lambda a: np.ascontiguousarray(np.asarray(a, dtype=np.float32))
    x, c, w_ada, b_ada, norm_attn, norm_ffn, w_in, w_out, rel_bias = map(f, (x, c, w_ada, b_ada, norm_attn, norm_ffn, w_in, w_out, rel_bias))
    peer_wq, peer_subkeys, peer_u, peer_v, norm_final = map(f, (peer_wq, peer_subkeys, peer_u, peer_v, norm_final))
    perm = tok_perm()
    nc = build_fused()
    cT = np.ascontiguousarray(c[0].reshape(8, 128).T)
    b_rows = rep(np.concatenate([b_ada[0], b_ada[1]]))
    rows = np.stack([rep(norm_attn[0]), rep(norm_attn[1]), rep(norm_ffn[0]), rep(norm_ffn[1]), rep(norm_final)])
    skT = np.stack([np.ascontiguousarray(peer_subkeys[l].reshape(16, 128, 128).transpose(2, 0, 1).reshape(128, 2048)) for l in range(2)])
    uT = np.ascontiguousarray(peer_u.transpose(0, 2, 1))
    idb = np.eye(128, dtype=np.float32).astype(NPBF)
    idf = np.eye(128, dtype=np.float32)
    st = np.arange(128)
    r_, j_ = st // 16, st % 16
    bs_of_st = (r_ // 2) * 16 + j_
    Eoh = (np.repeat(bs_of_st, 128)[None, :] == np.arange(64)[:, None]).astype(np.float32).astype(NPBF)
    t31m = np.ascontiguousarray(np.broadcast_to(rel_bias[None, 0:8, 31], (128, 8))).astype(np.float32)
    t31d = np.ascontiguousarray(np.broadcast_to(rel_bias[None, 8:16, 31], (128, 8))).astype(np.float32)
    shared = {"cT": cT, "w_ada": w_ada, "b_rows": b_rows, "rows": rows, "w_in": w_in, "w_out": w_out, "wq": peer_wq, "skT": skT,
              "uT": uT, "v": peer_v, "idb": idb, "idf": idf, "Eoh": Eoh, "t31m": t31m, "t31d": t31d}
    maps = []
    for i in range(NCORES):
        FM, SM, SA, FAR = moba_consts(i)
        pb_ = lambda a: np.ascontiguousarray(np.broadcast_to(perm_bs(a[0].reshape(CPC, 64)).reshape(1, -1), (128, CPC * 64)))
        m = dict(shared)
        m.update({"x": np.ascontiguousarray(x[0][perm[i]]),
                  "tzm": toeplitz_tables(rel_bias[0:8], i).reshape(8, 128, NDELTA * 128),
                  "tzd": toeplitz_tables(rel_bias[8:16], i).reshape(8, 128, NDELTA * 128),
                  "cFM": pb_(FM), "cSM": pb_(SM), "cSA": pb_(SA), "cFAR": pb_(FAR), "cCM": dsa_consts(i)})
        maps.append(m)
    res = run_bass_kernel_spmd(nc, maps, core_ids=list(range(NCORES)))
    outp = np.empty((S, D), np.float32)
    for i in range(NCORES):
        outp[perm[i]] = res.results[i]["out"]
    return outp[None].astype(np.float32)


def kernel_unfused(x, c, w_ada, b_ada, norm_attn, norm_ffn, w_in, w_out, rel_bias,
           peer_wq, peer_subkeys, peer_u, peer_v, norm_final):
    f = lambda a: np.asarray(a, dtype=np.float32)
    x, c, w_ada, b_ada, norm_attn, norm_ffn, w_in, w_out, rel_bias = map(f, (x, c, w_ada, b_ada, norm_attn, norm_ffn, w_in, w_out, rel_bias))
    peer_wq, peer_subkeys, peer_u, peer_v, norm_final = map(f, (peer_wq, peer_subkeys, peer_u, peer_v, norm_final))
    perm = tok_perm()
    mod = run_M(c[0], w_ada, b_ada)
    uT_all = run_cast(np.concatenate([peer_u[0].T, peer_u[1].T], axis=0))
    v_all = run_cast(np.concatenate([peer_v[0], peer_v[1]], axis=0))
    xc = x[0]
    for l in range(2):
        sh1, sc1, g1, sh2, sc2, g2 = (mod[l, i] for i in range(6))
        pf, pb = run_A(xc, norm_attn[l], sc1, sh1, w_in[l])
        ym = run_B(pf, pb, rel_bias)
        yd = run_C(pf, pb, rel_bias)
        yTs = [np.concatenate([ym[i].reshape(512, TPC), yd[i].reshape(512, TPC)], axis=0) for i in range(NCORES)]
        d1 = run_D1(xc, yTs, w_out[l], peer_wq[l], peer_subkeys[l], g1, norm_ffn[l], sc2, sh2)
        xc = run_D2(d1, np.ascontiguousarray(uT_all[l * 1024:(l + 1) * 1024]), np.ascontiguousarray(v_all[l * 16384:(l + 1) * 16384]),
                    g2, norm_final, final=(l == 1))
    return xc[None].astype(np.float32)
```
